# Optimizing a Trainium2 kernel written in Bass

```python
import jax, jax.numpy as jnp
from jax import lax
import numpy as np

D_MODEL = 1024
BATCH = 16
SEQ = 2048
DEPTH = 2

FOX_HEADS = 8
FOX_HEAD_DIM = 64
FOX_WIDTH = FOX_HEADS * FOX_HEAD_DIM
FOX_Q_BLOCK = 128
MOBA_HEADS = 8
MOBA_HEAD_DIM = 64
MOBA_WIDTH = MOBA_HEADS * MOBA_HEAD_DIM
MOBA_BLOCK = 256
MOBA_TOPK = 3
MOBA_Q_CHUNK = 16
MLSTM_HEADS = 4
MLSTM_QK_DIM = 64
MLSTM_V_DIM = 128
MLSTM_QK_WIDTH = MLSTM_HEADS * MLSTM_QK_DIM
MLSTM_V_WIDTH = MLSTM_HEADS * MLSTM_V_DIM
MLSTM_CHUNK = 64
MLSTM_CONV = 4
N_BRANCHES = 3
BRANCH_WIDTH = 512
RMS_EPS = 1e-6
NEG = -1e30

SPLIT_SIZES = (
    FOX_WIDTH, FOX_WIDTH, FOX_WIDTH, FOX_WIDTH, FOX_HEADS,
    MOBA_WIDTH, MOBA_WIDTH, MOBA_WIDTH, MOBA_WIDTH,
    2 * MLSTM_QK_WIDTH, MLSTM_V_WIDTH, MLSTM_V_WIDTH, MLSTM_V_WIDTH,
    MLSTM_HEADS, MLSTM_HEADS,
    N_BRANCHES * D_MODEL,
)
IN_WIDTH = int(sum(SPLIT_SIZES))
SPLIT_POINTS = tuple(int(v) for v in np.cumsum(SPLIT_SIZES)[:-1])

kernel_name = "hybrid_fox_moba_mlstm_gated_parallel"


def rms_norm(x, g):
    xf = x.astype(jnp.float32)
    y = xf * lax.rsqrt(jnp.mean(xf * xf, axis=-1, keepdims=True) + RMS_EPS)
    return (y * g.astype(jnp.float32)).astype(x.dtype)


def head_rms_norm(y, g, n_heads):
    b, s, w = y.shape
    yf = y.astype(jnp.float32).reshape(b, s, n_heads, w // n_heads)
    yf = yf * lax.rsqrt(jnp.mean(yf * yf, axis=-1, keepdims=True) + RMS_EPS)
    return (yf.reshape(b, s, w) * g.astype(jnp.float32)).astype(y.dtype)


def to_heads(t, n_heads):
    b, s, w = t.shape
    return t.reshape(b, s, n_heads, w // n_heads).transpose(0, 2, 1, 3)


def from_heads(t):
    b, h, s, d = t.shape
    return t.transpose(0, 2, 1, 3).reshape(b, s, h * d)


def alibi_slopes(n_heads):
    return 2.0 ** (-8.0 * (jnp.arange(n_heads, dtype=jnp.float32) + 1.0) / n_heads)


def causal_depthwise_conv(x, w):
    kw = w.shape[0]
    s = x.shape[1]
    xp = jnp.pad(x, ((0, 0), (kw - 1, 0), (0, 0)))
    return sum(xp[:, j:j + s] * w[j] for j in range(kw))


def forgetting_attention(q, k, v, log_f):
    b, h, s, d = q.shape
    nqb = s // FOX_Q_BLOCK
    scale = d ** -0.5
    c = jnp.cumsum(log_f, axis=-1)
    k_pos = jnp.arange(s)
    qb = q.reshape(b, h, nqb, FOX_Q_BLOCK, d).transpose(2, 0, 1, 3, 4)
    cb = c.reshape(b, h, nqb, FOX_Q_BLOCK).transpose(2, 0, 1, 3)
    q_pos = k_pos.reshape(nqb, FOX_Q_BLOCK)

    def one_block(args):
        q_blk, c_blk, pos = args
        logits = jnp.einsum('bhqd,bhkd->bhqk', q_blk, k).astype(jnp.float32) * scale
        logits = logits + c_blk[..., :, None] - c[..., None, :]
        logits = jnp.where(pos[:, None] >= k_pos[None, :], logits, NEG)
        p = jax.nn.softmax(logits, axis=-1)
        return jnp.einsum('bhqk,bhkd->bhqd', p.astype(v.dtype), v)

    out = lax.map(one_block, (qb, cb, q_pos))
    return out.transpose(1, 2, 0, 3, 4).reshape(b, h, s, d)


def moba_attention(q, k, v, slopes):
    b, h, s, d = q.shape
    scale = d ** -0.5
    nb = -(-s // MOBA_BLOCK)
    pad = nb * MOBA_BLOCK - s
    k_p = jnp.pad(k, ((0, 0), (0, 0), (0, pad), (0, 0)))
    v_p = jnp.pad(v, ((0, 0), (0, 0), (0, pad), (0, 0)))
    k_blocks = k_p.reshape(b, h, nb, MOBA_BLOCK, d)
    v_blocks = v_p.reshape(b, h, nb, MOBA_BLOCK, d)
    k_mean = jnp.mean(k_blocks.astype(jnp.float32), axis=3).astype(k.dtype)
    topk = min(MOBA_TOPK, nb)
    n_chunks = s // MOBA_Q_CHUNK
    qc = q.reshape(b, h, n_chunks, MOBA_Q_CHUNK, d).transpose(2, 0, 1, 3, 4)
    blk_ids = jnp.arange(nb)
    offs = jnp.arange(MOBA_BLOCK)
    bi = jnp.arange(b)[:, None, None, None]
    hi = jnp.arange(h)[None, :, None, None]
    m = slopes.astype(jnp.float32)

    def one_chunk(args):
        q_c, c_idx = args
        start = c_idx * MOBA_Q_CHUNK
        q_pos = start + jnp.arange(MOBA_Q_CHUNK)
        own = start // MOBA_BLOCK
        gate = jnp.einsum('bhqd,bhnd->bhqn', q_c, k_mean).astype(jnp.float32)
        gate = jnp.where(blk_ids < own, gate, NEG)
        _, sel = lax.top_k(gate, topk)
        sel_valid = jnp.arange(topk) < own
        k_sel = k_blocks[bi, hi, sel]
        v_sel = v_blocks[bi, hi, sel]
        sel_pos = sel[..., None] * MOBA_BLOCK + offs
        s_sel = jnp.einsum('bhqd,bhqnkd->bhqnk', q_c, k_sel).astype(jnp.float32) * scale
        dist_sel = (q_pos[:, None, None] - sel_pos).astype(jnp.float32)
        s_sel = s_sel - m[None, :, None, None, None] * dist_sel
        s_sel = jnp.where(sel_valid[:, None], s_sel, NEG)
        k_own = lax.dynamic_slice_in_dim(k_p, own * MOBA_BLOCK, MOBA_BLOCK, axis=2)
        v_own = lax.dynamic_slice_in_dim(v_p, own * MOBA_BLOCK, MOBA_BLOCK, axis=2)
        own_pos = own * MOBA_BLOCK + offs
        s_own = jnp.einsum('bhqd,bhkd->bhqk', q_c, k_own).astype(jnp.float32) * scale
        dist_own = (q_pos[:, None] - own_pos[None, :]).astype(jnp.float32)
        s_own = s_own - m[None, :, None, None] * dist_own
        s_own = jnp.where(dist_own >= 0, s_own, NEG)
        nsel = topk * MOBA_BLOCK
        logits = jnp.concatenate(
            [s_sel.reshape(b, h, MOBA_Q_CHUNK, nsel), s_own], axis=-1)
        p = jax.nn.softmax(logits, axis=-1).astype(v.dtype)
        p_sel = p[..., :nsel].reshape(b, h, MOBA_Q_CHUNK, topk, MOBA_BLOCK)
        p_own = p[..., nsel:]
        return (jnp.einsum('bhqnk,bhqnkd->bhqd', p_sel, v_sel)
                + jnp.einsum('bhqk,bhkd->bhqd', p_own, v_own))

    out = lax.map(one_chunk, (qc, jnp.arange(n_chunks)))
    return out.transpose(1, 2, 0, 3, 4).reshape(b, h, s, d)


def mlstm_chunkwise(q, k, v, log_i, log_f):
    b, h, s, dqk = q.shape
    dv = v.shape[-1]
    L = MLSTM_CHUNK
    nc = s // L
    k = k * (dqk ** -0.5)

    def chunks(t):
        return jnp.moveaxis(t.reshape(b, h, nc, L, *t.shape[3:]), 2, 0)

    xs = tuple(chunks(t) for t in (q, k, v, log_i, log_f))
    tri = jnp.tril(jnp.ones((L, L), dtype=bool))

    def step(carry, inp):
        C, n, m = carry
        q_t, k_t, v_t, li, lf = inp
        qf = q_t.astype(jnp.float32)
        kf = k_t.astype(jnp.float32)
        vf = v_t.astype(jnp.float32)
        bcum = jnp.cumsum(lf, axis=-1)
        d_intra = jnp.where(tri, bcum[..., :, None] - bcum[..., None, :] + li[..., None, :], NEG)
        d_inter = bcum + m[..., None]
        m_t = jnp.maximum(d_inter, jnp.max(d_intra, axis=-1))
        w_intra = jnp.exp(d_intra - m_t[..., None])
        w_inter = jnp.exp(d_inter - m_t)
        qk = jnp.einsum('bhtd,bhsd->bhts', qf, kf) * w_intra
        num = (jnp.einsum('bhts,bhsv->bhtv', qk, vf)
               + w_inter[..., None] * jnp.einsum('bhvd,bhtd->bhtv', C, qf))
        den = jnp.sum(qk, axis=-1) + w_inter * jnp.einsum('bhd,bhtd->bht', n, qf)
        h_t = num / jnp.maximum(jnp.abs(den), jnp.exp(-m_t))[..., None]
        b_last = bcum[..., -1]
        d_state = b_last[..., None] - bcum + li
        m_new = jnp.maximum(b_last + m, jnp.max(d_state, axis=-1))
        w_prev = jnp.exp(b_last + m - m_new)
        w_s = jnp.exp(d_state - m_new[..., None])
        C_new = w_prev[..., None, None] * C + jnp.einsum('bhs,bhsv,bhsd->bhvd', w_s, vf, kf)
        n_new = w_prev[..., None] * n + jnp.einsum('bhs,bhsd->bhd', w_s, kf)
        return (C_new, n_new, m_new), h_t

    init = (jnp.zeros((b, h, dv, dqk), jnp.float32),
            jnp.zeros((b, h, dqk), jnp.float32),
            jnp.zeros((b, h), jnp.float32))
    _, hs = lax.scan(step, init, xs)
    return jnp.moveaxis(hs, 0, 2).reshape(b, h, s, dv).astype(v.dtype)


def hybrid_layer(x, norm_g, w_in, fox_b_f, mlstm_conv_w, mlstm_b_i, mlstm_b_f,
                 mlstm_head_g, w_branch, w_out, slopes):
    b, s, _ = x.shape
    hn = rms_norm(x, norm_g)
    proj = jnp.einsum('bsd,dc->bsc', hn, w_in)
    (a_q, a_k, a_v, a_z, a_f, b_q, b_k, b_v, b_z,
     c_qk, c_v, c_o, c_z, c_i, c_f, gates) = jnp.split(proj, SPLIT_POINTS, axis=-1)

    log_f_a = jax.nn.log_sigmoid((a_f + fox_b_f).astype(jnp.float32)).transpose(0, 2, 1)
    y_a = forgetting_attention(to_heads(a_q, FOX_HEADS), to_heads(a_k, FOX_HEADS),
                               to_heads(a_v, FOX_HEADS), log_f_a)
    y_a = from_heads(y_a) * jax.nn.silu(a_z)

    y_b = moba_attention(to_heads(b_q, MOBA_HEADS), to_heads(b_k, MOBA_HEADS),
                         to_heads(b_v, MOBA_HEADS), slopes)
    y_b = from_heads(y_b) * jax.nn.silu(b_z)

    qk = jax.nn.silu(causal_depthwise_conv(c_qk, mlstm_conv_w))
    c_q, c_k = jnp.split(qk, 2, axis=-1)
    log_i = (c_i + mlstm_b_i).astype(jnp.float32).transpose(0, 2, 1)
    log_f_c = jax.nn.log_sigmoid((c_f + mlstm_b_f).astype(jnp.float32)).transpose(0, 2, 1)
    h_c = mlstm_chunkwise(to_heads(c_q, MLSTM_HEADS), to_heads(c_k, MLSTM_HEADS),
                          to_heads(c_v, MLSTM_HEADS), log_i, log_f_c)
    h_c = from_heads(h_c) * jax.nn.sigmoid(c_o)
    y_c = head_rms_norm(h_c, mlstm_head_g, MLSTM_HEADS) * jax.nn.silu(c_z)

    ys = jnp.stack([y_a, y_b, y_c], axis=2)
    branch_out = jnp.einsum('bsnw,nwd->bsnd', ys, w_branch)
    g = jax.nn.sigmoid(gates).reshape(b, s, N_BRANCHES, D_MODEL)
    merged = jnp.sum(g * branch_out, axis=2)
    return x + jnp.einsum('bsd,de->bse', merged, w_out)


def setup_inputs(seed: int = 0) -> dict:
    key = jax.random.key(seed)
    ks = jax.random.split(key, 12)
    f32 = jnp.float32
    x = jax.random.normal(ks[0], (BATCH, SEQ, D_MODEL), f32)
    norm_g = 1.0 + 0.02 * jax.random.normal(ks[1], (DEPTH, D_MODEL), f32)
    w_in = jax.random.normal(ks[2], (DEPTH, D_MODEL, IN_WIDTH), f32) * D_MODEL ** -0.5
    fox_b_f = (jnp.linspace(1.0, 4.0, FOX_HEADS, dtype=f32)[None, :]
               + 0.05 * jax.random.normal(ks[3], (DEPTH, FOX_HEADS), f32))
    mlstm_conv_w = jax.random.normal(ks[4], (DEPTH, MLSTM_CONV, 2 * MLSTM_QK_WIDTH), f32) * MLSTM_CONV ** -0.5
    mlstm_b_i = 0.1 * jax.random.normal(ks[5], (DEPTH, MLSTM_HEADS), f32)
    mlstm_b_f = (jnp.linspace(3.0, 6.0, MLSTM_HEADS, dtype=f32)[None, :]
                 + 0.05 * jax.random.normal(ks[6], (DEPTH, MLSTM_HEADS), f32))
    mlstm_head_g = 1.0 + 0.02 * jax.random.normal(ks[7], (DEPTH, MLSTM_V_WIDTH), f32)
    w_branch = jax.random.normal(ks[8], (DEPTH, N_BRANCHES, BRANCH_WIDTH, D_MODEL), f32) * BRANCH_WIDTH ** -0.5
    w_out = jax.random.normal(ks[9], (DEPTH, D_MODEL, D_MODEL), f32) * (0.5 * D_MODEL ** -0.5)
    final_norm_g = 1.0 + 0.02 * jax.random.normal(ks[10], (D_MODEL,), f32)
    return {"x": x, "norm_g": norm_g, "w_in": w_in, "fox_b_f": fox_b_f,
            "mlstm_conv_w": mlstm_conv_w, "mlstm_b_i": mlstm_b_i, "mlstm_b_f": mlstm_b_f,
            "mlstm_head_g": mlstm_head_g, "w_branch": w_branch, "w_out": w_out,
            "final_norm_g": final_norm_g}


def reference(x, norm_g, w_in, fox_b_f, mlstm_conv_w, mlstm_b_i, mlstm_b_f,
              mlstm_head_g, w_branch, w_out, final_norm_g):
    slopes = alibi_slopes(MOBA_HEADS)
    for layer in range(DEPTH):
        x = hybrid_layer(x, norm_g[layer], w_in[layer], fox_b_f[layer], mlstm_conv_w[layer],
                         mlstm_b_i[layer], mlstm_b_f[layer], mlstm_head_g[layer],
                         w_branch[layer], w_out[layer], slopes)
    return rms_norm(x, final_norm_g)
```

```python
import contextlib
import numpy as np
import concourse.bass as bass
import concourse.mybir as mybir
from concourse.bass_utils import run_bass_kernel_spmd

F32 = mybir.dt.float32
BF16 = mybir.dt.bfloat16
U8 = mybir.dt.uint8
AF = mybir.ActivationFunctionType
ALU = mybir.AluOpType
AX = mybir.AxisListType

S = 2048
D = 1024
NT = 16
NCH = 4
DEPTH = 2
NSEQ = 2
INW = 9232
OFF = dict(a_q=0, a_k=512, a_v=1024, a_z=1536, a_f=2048, b_q=2056, b_k=2568, b_v=3080,
           b_z=3592, c_qk=4104, c_v=4616, c_o=5128, c_z=5640, c_if=6152, gates=6160)
EPS = 1e-6
BIG = 30000.0


class Buf:
    __slots__ = ("name", "last_w", "reads")

    def __init__(self, name):
        self.name = name
        self.last_w = None
        self.reads = []


class Op:
    __slots__ = ("eng", "fn", "deps", "signal", "sigval", "dma_ch", "dma_n", "epoch", "idx")


class Prog:
    ENGS = ("tensor", "vector", "scalar", "gpsimd", "sync")

    def __init__(self, nc):
        self.nc = nc
        self.q = {e: [] for e in self.ENGS}
        self.ops = []
        self.epoch = 0
        self.last_real = {e: None for e in self.ENGS}
        self.last_dma = {}
        self.channels = []

    def op(self, eng, fn, reads=(), writes=(), dma_ch=None, dma_n=1, extra=()):
        o = Op()
        o.eng = eng
        o.fn = fn
        o.signal = False
        o.sigval = None
        o.dma_ch = dma_ch
        o.dma_n = dma_n
        o.epoch = self.epoch
        o.idx = len(self.ops)
        deps = set(extra)
        for b in reads:
            if b.last_w is not None:
                deps.add(b.last_w)
        for b in writes:
            if b.last_w is not None:
                deps.add(b.last_w)
            deps.update(b.reads)
        deps.discard(o)
        best = {}
        for d in deps:
            if d.fn is None:
                continue
            if d.dma_ch is not None:
                key = ("dma", d.dma_ch)
            else:
                if d.eng == "tensor" and eng == "tensor" and dma_ch is None:
                    continue
                key = ("eng", d.eng, d.epoch)
            if key not in best or best[key].idx < d.idx:
                best[key] = d
        o.deps = list(best.values())
        for b in reads:
            b.reads.append(o)
        for b in writes:
            b.last_w = o
            b.reads = []
        self.q[eng].append(o)
        self.ops.append(o)
        if fn is not None:
            if dma_ch is not None:
                self.last_dma[dma_ch] = o
                if dma_ch not in self.channels:
                    self.channels.append(dma_ch)
            else:
                self.last_real[eng] = o
        return o

    def barrier(self, new_epoch=False):
        tgt = [o for o in self.last_real.values() if o is not None] + list(self.last_dma.values())
        for e in self.ENGS:
            self.op(e, None, extra=tgt)
        if new_epoch:
            self.epoch += 1

    def emit(self, final_waits=()):
        nc = self.nc
        for o in self.ops:
            for d in o.deps:
                d.signal = True
        for o in final_waits:
            o.signal = True
        with contextlib.ExitStack() as st:
            esem = {}
            for ep in range(self.epoch + 1):
                for e in self.ENGS:
                    esem[(ep, e)] = st.enter_context(nc.semaphore("s%d_%s" % (ep, e)))
            dsem = {c: st.enter_context(nc.semaphore("d_" + c)) for c in self.channels}
            cnt = {}
            chcnt = {}
            for e in self.ENGS:
                for o in self.q[e]:
                    if o.fn is None:
                        continue
                    if o.dma_ch is not None:
                        chcnt[o.dma_ch] = chcnt.get(o.dma_ch, 0) + 16 * o.dma_n
                        o.sigval = (dsem[o.dma_ch], chcnt[o.dma_ch])
                    elif o.signal:
                        k = (o.epoch, e)
                        cnt[k] = cnt.get(k, 0) + 1
                        o.sigval = (esem[k], cnt[k])
            prog = self

            def run(e, engine):
                seen = {}
                for o in prog.q[e]:
                    need = {}
                    for d in o.deps:
                        if d.sigval is None:
                            continue
                        s, v = d.sigval
                        k = id(s)
                        if seen.get(k, 0) >= v:
                            continue
                        if k not in need or need[k][1] < v:
                            need[k] = (s, v)
                    for k, (s, v) in need.items():
                        engine.wait_ge(s, v)
                        seen[k] = v
                    if o.fn is None:
                        continue
                    ins = o.fn(engine)
                    if o.dma_ch is not None:
                        lst = ins if isinstance(ins, (list, tuple)) else [ins]
                        assert len(lst) == o.dma_n, (len(lst), o.dma_n)
                        for i_ in lst:
                            i_.then_inc(o.sigval[0], 16)
                    elif o.signal:
                        ins.then_inc(o.sigval[0], 1)
                if e == "sync":
                    for o in final_waits:
                        s, v = o.sigval
                        engine.wait_ge(s, v)

            with nc.Block() as block:
                @block.tensor
                def _(eng):
                    run("tensor", eng)

                @block.vector
                def _(eng):
                    run("vector", eng)

                @block.scalar
                def _(eng):
                    run("scalar", eng)

                @block.gpsimd
                def _(eng):
                    run("gpsimd", eng)

                @block.sync
                def _(eng):
                    run("sync", eng)


def host_consts():
    c = {}
    c["ident"] = np.eye(128, dtype=np.float32)
    kk = np.arange(128)
    c["tri01"] = (kk[None, :] >= kk[:, None]).astype(np.float32)
    c["trif"] = (kk[:, None] <= kk[None, :]).astype(np.float32)
    pos = np.arange(S)
    ksd = np.zeros((10, 2, S), np.float32)
    for kb in range(8):
        ksd[kb, :, :] = (pos // 256 == kb).astype(np.float32)[None, :]
    ksd[8:10] = 1.0
    c["kside"] = ksd
    mt = np.zeros((128, NT, 8), np.float32)
    for t in range(NT):
        own = t // 2
        for kb in range(8):
            mt[:, t, kb] = 0.0 if kb < own else (1e30 if kb == own else -1e30)
    c["mtable"] = mt.reshape(128, NT * 8)
    slopes = 2.0 ** (-8.0 * (np.arange(8) + 1.0) / 8)
    tokpos = (np.arange(NT)[None, :] * 128 + np.arange(128)[:, None]).astype(np.float64)
    hi = np.floor(tokpos / 256) * 256
    lo = tokpos - hi
    al = np.zeros((128, NT, 8, 2), np.float32)
    kb_ = np.zeros((128, NT, 8), np.float32)
    for h in range(8):
        al[:, :, h, 0] = -slopes[h] * hi
        al[:, :, h, 1] = -slopes[h] * lo
        kb_[:, :, h] = slopes[h] * tokpos
    c["alibi_q"] = al.reshape(128, NT * 8 * 2)
    c["alibi_k"] = kb_.reshape(128, NT * 8)
    return c


CONST_SHAPES = dict(ident=(128, 128), tri01=(128, 128), trif=(128, 128), kside=(10, 2, S),
                    mtable=(128, 128), alibi_q=(128, 256), alibi_k=(128, 128))


def build(dbg=False):
    nc = bass.Bass("TRN2", target_bir_lowering=False)

    def din(name, shape):
        return nc.dram_tensor(name, list(shape), F32, kind="ExternalInput").ap()

    x_in = din("x", (NSEQ, S, D))
    w_in = din("w_in", (DEPTH, D, INW))
    w_br = din("w_branch", (DEPTH, 3, 512, D))
    w_out = din("w_out", (DEPTH, D, D))
    gfull_d = din("gfull", (128, DEPTH * 8 * 128))
    fgrep_d = din("fgrep", (128, D))
    fbf_d = din("fbf_rep", (128, DEPTH * 128))
    bif_d = din("bif_rep", (128, DEPTH * 128))
    cw_d = din("cw", (128, DEPTH * 16))
    hg_d = din("hg", (128, DEPTH * 4))
    cd = {k: din("c_" + k, v) for k, v in CONST_SHAPES.items()}
    out_d = nc.dram_tensor("out", [NSEQ, S, D], F32, kind="ExternalOutput").ap()
    xs_d = nc.dram_tensor("xs_scratch", [NSEQ, S, D], F32, kind="Internal").ap()
    dbg_d = {}
    if dbg:
        for nm in ("dbg_hnT",):
            dbg_d[nm] = nc.dram_tensor(nm, [128, 8 * S], F32, kind="ExternalOutput").ap()
        for nm in ("dbg_ya", "dbg_yb", "dbg_yc"):
            dbg_d[nm] = nc.dram_tensor(nm, [128, 4 * S], F32, kind="ExternalOutput").ap()
        dbg_d["dbg_x1"] = nc.dram_tensor("dbg_x1", [S, D], F32, kind="ExternalOutput").ap()

    TOTAL = 206000
    big = nc.alloc_sbuf_tensor("big", [128, TOTAL], U8)
    cur = [0]

    def alloc(nbytes):
        o = cur[0]
        cur[0] = o + ((nbytes + 63) // 64) * 64
        assert cur[0] <= TOTAL, cur[0]
        return o

    def view(off, n, dt):
        sz = 4 if dt == F32 else 2
        return big[:, off:off + n * sz].bitcast(dt)

    def tl(n, dt):
        sz = 4 if dt == F32 else 2
        return view(alloc(n * sz), n, dt)

    hnT = tl(8 * S, BF16).rearrange("p (c n) -> p c n", c=8)
    yT = [tl(4 * S, BF16).rearrange("p (c n) -> p c n", c=4) for _ in range(3)]
    ident = tl(128, BF16)
    tri01 = tl(128, BF16)
    onesb = tl(128, BF16)
    o128b = tl(128, BF16)
    trif = tl(128, F32)
    onesf = tl(128, F32)
    gfull = tl(DEPTH * 8 * 128, BF16).rearrange("p (l c j) -> p l c j", l=DEPTH, c=8)
    fgrep = tl(D, F32)
    fbf = tl(DEPTH * 128, F32).rearrange("p (l n) -> p l n", l=DEPTH)
    bif = tl(DEPTH * 128, F32).rearrange("p (l n) -> p l n", l=DEPTH)
    cw = tl(DEPTH * 16, F32).rearrange("p (l c j) -> p l c j", l=DEPTH, c=4)
    hg = tl(DEPTH * 4, F32).rearrange("p (l h) -> p l h", l=DEPTH)
    mtable = tl(128, F32)
    alibi_k = tl(128, F32)
    epsc = tl(1, F32)
    stage0 = cur[0]

    ps = [nc.alloc_psum_tensor("ps%d" % i, [128, 512], F32)[:, :] for i in range(8)]
    bps = [Buf("ps%d" % i) for i in range(8)]
    psb = [ps[7], ps[6]]
    bpsb = [bps[7], bps[6]]

    P = Prog(nc)
    b_hnT = [Buf("hnT%d" % i) for i in range(NCH)]
    b_yT = [[[Buf("y") for _ in range(NCH)] for _ in range(4)] for _ in range(3)]
    b_const = Buf("const")

    def ld(dst, src):
        q = "sync" if dst.dtype == src.dtype else "gpsimd"
        P.op(q, lambda e: e.dma_start(out=dst, in_=src), writes=[b_const], dma_ch="const_" + q)

    ld(ident, cd["ident"])
    ld(tri01, cd["tri01"])
    ld(trif, cd["trif"])
    ld(gfull.rearrange("p l c j -> p (l c j)"), gfull_d)
    ld(fgrep, fgrep_d)
    ld(fbf.rearrange("p l n -> p (l n)"), fbf_d)
    ld(bif.rearrange("p l n -> p (l n)"), bif_d)
    ld(cw.rearrange("p l c j -> p (l c j)"), cw_d)
    ld(hg.rearrange("p l h -> p (l h)"), hg_d)
    ld(mtable, cd["mtable"])
    ld(alibi_k, cd["alibi_k"])
    P.op("gpsimd", lambda e: e.memset(onesb, 1.0), writes=[b_const])
    P.op("gpsimd", lambda e: e.memset(o128b, 1.0 / 128.0), writes=[b_const])
    P.op("gpsimd", lambda e: e.memset(onesf, 1.0), writes=[b_const])
    P.op("gpsimd", lambda e: e.memset(epsc, EPS), writes=[b_const])
    P.barrier()

    RC = [b_const]
    psrr = [0]

    def proj_fm(wt, bw, j0, chunk, pidx):
        for c in range(8):
            P.op("tensor", lambda e, c=c: e.matmul(ps[pidx][:, :], lhsT=wt[:, c, j0:j0 + 128],
                                                   rhs=hnT[:, c, chunk * 512:(chunk + 1) * 512],
                                                   start=(c == 0), stop=(c == 7)),
                 reads=[bw, b_hnT[chunk]], writes=[bps[pidx]])

    def wload(wt, bw, ch, l, cols, eng="gpsimd"):
        src = w_in[l].rearrange("(c p) n -> p c n", p=128)

        def fn(e):
            return [e.dma_start(out=wt[:, :, d0:d0 + n], in_=src[:, :, c0:c0 + n]) for (c0, n, d0) in cols]
        P.op(eng, fn, writes=[bw], dma_ch=ch, dma_n=len(cols))

    def norm_stage(s, l):
        cur[0] = stage0
        xt = [tl(D, F32) for _ in range(2)]
        hnb = [tl(D, BF16) for _ in range(2)]
        ss = tl(NT, F32)
        rstd = tl(NT, F32)
        bxt = [Buf("xt0"), Buf("xt1")]
        bhnb = [Buf("hnb0"), Buf("hnb1")]
        bss = Buf("ss")
        src = x_in[s] if l == 0 else xs_d[s]
        import os
        ncut = int(os.environ.get("MK_NCUT", "99"))
        for t in range(NT):
            i = t % 2
            P.op("sync", lambda e, i=i, t=t: e.dma_start(out=xt[i], in_=src[t * 128:(t + 1) * 128, :]),
                 writes=[bxt[i]], dma_ch="xt%d" % i)
            if ncut < 1:
                continue
            P.op("scalar", lambda e, i=i, t=t: e.activation(out=hnb[i], in_=xt[i], func=AF.Square,
                                                            accum_out=ss[:, t:t + 1]),
                 reads=[bxt[i]], writes=[bhnb[i], bss])
            if ncut < 2:
                continue
            P.op("scalar", lambda e, t=t: e.activation(out=rstd[:, t:t + 1], in_=ss[:, t:t + 1], func=AF.Ln,
                                                       bias=epsc, scale=1.0 / D),
                 reads=[bss] + RC, writes=[bss])
            P.op("scalar", lambda e, t=t: e.activation(out=rstd[:, t:t + 1], in_=rstd[:, t:t + 1], func=AF.Exp,
                                                       scale=-0.5),
                 reads=[bss], writes=[bss])
            if ncut < 3:
                continue
            P.op("scalar", lambda e, i=i, t=t: e.activation(out=hnb[i], in_=xt[i], func=AF.Copy,
                                                            scale=rstd[:, t:t + 1]),
                 reads=[bxt[i], bss], writes=[bhnb[i]])
            if ncut < 4:
                continue
            for hf in range(2):
                for cc in range(4):
                    c = hf * 4 + cc
                    P.op("tensor", lambda e, i=i, c=c, cc=cc, hf=hf: e.matmul(
                        psb[hf][:, cc * 128:(cc + 1) * 128], lhsT=hnb[i][:, c * 128:(c + 1) * 128], rhs=ident,
                        start=True, stop=True),
                        reads=[bhnb[i]] + RC, writes=[bpsb[hf]])
                eng = "vector"
                if ncut < 5:
                    continue
                P.op(eng, lambda e, hf=hf, t=t: e.tensor_tensor(
                    out=hnT[:, hf * 4:(hf + 1) * 4, t * 128:(t + 1) * 128],
                    in0=psb[hf].rearrange("p (c j) -> p c j", c=4),
                    in1=gfull[:, l, hf * 4:(hf + 1) * 4, :], op=ALU.mult),
                    reads=[bpsb[hf]] + RC, writes=[b_hnT[t // 4]])
        P.barrier()

    def attn_stage(s, l, br):
        cur[0] = stage0
        pre = "a_" if br == 0 else "b_"
        qaug = tl(2 * S, BF16).rearrange("p (h n) -> p h n", h=2)
        kaug = tl(2 * S, BF16).rearrange("p (h n) -> p h n", h=2)
        zs = tl(S, BF16)
        vaug = tl(NT * 768, BF16).rearrange("p (t n) -> p t n", t=NT)
        wv = tl(8 * 512, BF16).rearrange("p (c n) -> p c n", c=8)
        wf = tl(8 * 8, BF16).rearrange("p (c n) -> p c n", c=8)
        wp = [tl(8 * 384, BF16).rearrange("p (c n) -> p c n", c=8) for _ in range(2)]
        pt = [tl(512, BF16) for _ in range(4)]
        ext = tl(NT * 8 * 10, BF16).rearrange("p (t h e) -> p t h e", t=NT, h=8)
        gk = tl(128, F32)
        u1 = tl(128, F32)
        u2 = tl(128, F32)
        gp = tl(128, F32)
        srt = tl(128, F32)
        thr = tl(NT, F32)
        km = tl(16, F32)
        kmb = tl(16, BF16)
        rr = [tl(512, F32) for _ in range(2)]
        tm = [tl(512, F32) for _ in range(2)]
        b_q = [[Buf("q") for _ in range(NCH)] for _ in range(2)]
        b_k = [[Buf("k") for _ in range(NCH)] for _ in range(2)]
        b_z = [Buf("z") for _ in range(NCH)]
        b_v = [Buf("v") for _ in range(NT)]
        b_wv, b_wf = Buf("wv"), Buf("wf")
        b_wp = [Buf("wp0"), Buf("wp1")]
        b_pt = [Buf("pt") for _ in range(4)]
        b_ext = Buf("ext")
        b_sm = Buf("small")
        b_km = Buf("km")
        b_rr = [Buf("rr0"), Buf("rr1")]
        b_tm = [Buf("tm0"), Buf("tm1")]
        yt = yT[br]
        byt = b_yT[br]

        b_kc = Buf("kconst")
        P.op("gpsimd", lambda e: e.dma_start(out=kaug[64:74, :, :], in_=cd["kside"]), writes=[b_kc], dma_ch="kconst")
        P.op("gpsimd", lambda e: e.memset(vaug.rearrange("p t n -> p (t n)"), 1.0), writes=b_v)
        P.op("gpsimd", lambda e: e.memset(ext.rearrange("p t h e -> p (t h e)"), 0.0), writes=[b_ext])
        if br == 1:
            alq = tl(256, BF16)
            b_alq = Buf("alq")
            P.op("gpsimd", lambda e: e.dma_start(out=alq, in_=cd["alibi_q"]), writes=[b_alq], dma_ch="alq")
            P.op("gpsimd", lambda e: e.tensor_copy(out=ext[:, :, :, 8:10],
                                                   in_=alq.rearrange("p (t h e) -> p t h e", t=NT, h=8)),
                 reads=[b_alq], writes=[b_ext])
        wload(wv, b_wv, "wv", l, [(OFF[pre + "v"], 512, 0)])
        if br == 0:
            wload(wf, b_wf, "wf", l, [(OFF["a_f"], 8, 0)])

        def load_pair(j):
            wload(wp[j % 2], b_wp[j % 2], "wp%d" % (j % 2), l,
                  [(OFF[pre + "q"] + j * 128, 128, 0), (OFF[pre + "k"] + j * 128, 128, 128),
                   (OFF[pre + "z"] + j * 128, 128, 256)])
        load_pair(0)

        for t in range(NT):
            pi = t % 2
            for c in range(8):
                P.op("tensor", lambda e, c=c, t=t, pi=pi: e.matmul(
                    ps[pi][:, :], lhsT=hnT[:, c, t * 128:(t + 1) * 128], rhs=wv[:, c, :],
                    start=(c == 0), stop=(c == 7)), reads=[b_wv, b_hnT[t // 4]], writes=[bps[pi]])
            vv = vaug[:, t, :].rearrange("p (j n) -> p j n", j=4)
            pv = ps[pi].rearrange("p (j hh d) -> p j hh d", j=4, hh=2)
            P.op("scalar", lambda e, vv=vv, pv=pv: e.activation(out=vv[:, :, 0:64], in_=pv[:, :, 0, :], func=AF.Copy),
                 reads=[bps[pi]], writes=[b_v[t]])
            P.op("vector", lambda e, vv=vv, pv=pv: e.tensor_copy(out=vv[:, :, 128:192], in_=pv[:, :, 1, :]),
                 reads=[bps[pi]], writes=[b_v[t]])

        if br == 0:
            for t in range(NT):
                for c in range(8):
                    P.op("tensor", lambda e, c=c, t=t: e.matmul(
                        ps[6][:, t * 8:(t + 1) * 8], lhsT=hnT[:, c, t * 128:(t + 1) * 128], rhs=wf[:, c, :],
                        start=(c == 0), stop=(c == 7)), reads=[b_wf, b_hnT[t // 4]], writes=[bps[6]])
            P.op("vector", lambda e: e.tensor_tensor(out=u1, in0=ps[6][:, 0:128], in1=fbf[:, l, :], op=ALU.add),
                 reads=[bps[6]] + RC, writes=[b_sm])
            P.op("scalar", lambda e: e.activation(out=u2, in_=u1, func=AF.Exp, scale=-1.0), reads=[b_sm], writes=[b_sm])
            P.op("vector", lambda e: e.tensor_scalar_add(out=u2, in0=u2, scalar1=1.0), reads=[b_sm], writes=[b_sm])
            P.op("scalar", lambda e: e.activation(out=u1, in_=u2, func=AF.Ln), reads=[b_sm], writes=[b_sm])
            for t in range(NT):
                for tp in range(t + 1):
                    P.op("tensor", lambda e, t=t, tp=tp: e.matmul(
                        ps[5][:, t * 8:(t + 1) * 8], lhsT=(trif if tp == t else onesf),
                        rhs=u1[:, tp * 8:(tp + 1) * 8], start=(tp == 0), stop=(tp == t)),
                        reads=[b_sm] + RC, writes=[bps[5]])
            P.op("vector", lambda e: e.tensor_copy(out=gk, in_=ps[5][:, 0:128]), reads=[bps[5]], writes=[b_sm])
            gv = gk.rearrange("p (t h) -> p t h", t=NT)
            P.op("vector", lambda e: e.tensor_scalar(out=ext[:, :, :, 8], in0=gv, scalar1=-1.0, scalar2=None,
                                                     op0=ALU.mult), reads=[b_sm], writes=[b_ext])
            P.op("vector", lambda e: e.scalar_tensor_tensor(out=ext[:, :, :, 9], in0=gv, scalar=-1.0,
                                                            in1=ext[:, :, :, 8], op0=ALU.mult, op1=ALU.subtract),
                 reads=[b_sm, b_ext], writes=[b_ext])
            biast = gk
        else:
            biast = alibi_k

        def attention(hh, h):
            j = h // 2
            lo, hi_ = (0, 64) if hh == 0 else (64, 128)
            llo = 64 if hh == 0 else 0
            vc0 = j * 192 + (0 if hh == 0 else 64)
            tiles = []
            for qc in range(NCH):
                for kt in range(4 * qc + 4):
                    tiles.append((qc, kt))
            pend = []
            pidx = [0]

            def issue_pv(item):
                qc, kt, c0, n, pti = item
                po = 4 + (qc % 2)
                P.op("tensor", lambda e: e.matmul(
                    ps[po][:, c0:c0 + n], lhsT=vaug[:, kt, vc0:vc0 + 128], rhs=pt[pti][:, 0:n],
                    start=(kt == 0), stop=(kt == 4 * qc + 3)),
                    reads=[b_v[kt], b_pt[pti]], writes=[bps[po]])
                if kt == 4 * qc + 3:
                    fin(qc, po)

            def fin(qc, po):
                ri = qc % 2
                P.op("vector", lambda e: e.reciprocal(out=rr[ri][lo:hi_, :], in_=ps[po][llo:llo + 64, :]),
                     reads=[bps[po]], writes=[b_rr[ri]])
                P.op("vector", lambda e: e.tensor_tensor(out=tm[ri][lo:hi_, :], in0=ps[po][lo:hi_, :],
                                                         in1=rr[ri][lo:hi_, :], op=ALU.mult),
                     reads=[bps[po], b_rr[ri]], writes=[b_tm[ri]])
                P.op("gpsimd", lambda e: e.tensor_tensor(out=yt[lo:hi_, j, qc * 512:(qc + 1) * 512],
                                                         in0=tm[ri][lo:hi_, :], in1=zs[lo:hi_, qc * 512:(qc + 1) * 512],
                                                         op=ALU.mult),
                     reads=[b_tm[ri], b_z[qc]], writes=[byt[j][qc]])

            for (qc, kt) in tiles:
                d = kt - 4 * qc
                c0 = 0 if d < 0 else d * 128
                n = 512 - c0
                si = 2 + (pidx[0] % 2)
                pti = pidx[0] % 4
                pidx[0] += 1
                q0 = qc * 512 + c0
                P.op("tensor", lambda e, kt=kt, q0=q0, n=n, si=si: e.matmul(
                    ps[si][:, 0:n], lhsT=kaug[0:74, hh, kt * 128:(kt + 1) * 128], rhs=qaug[0:74, hh, q0:q0 + n],
                    start=True, stop=True),
                    reads=[b_k[hh][kt // 4], b_q[hh][qc], b_kc], writes=[bps[si]])
                P.op("scalar", lambda e, kt=kt, n=n, si=si, pti=pti: e.activation(
                    out=pt[pti][:, 0:n], in_=ps[si][:, 0:n], func=AF.Exp, bias=biast[:, kt * 8 + h:kt * 8 + h + 1]),
                    reads=[bps[si], b_sm] + RC, writes=[b_pt[pti]])
                if d >= 0:
                    P.op("gpsimd", lambda e, pti=pti: e.tensor_tensor(out=pt[pti][:, 0:128], in0=pt[pti][:, 0:128],
                                                                      in1=tri01, op=ALU.mult),
                         reads=[b_pt[pti]] + RC, writes=[b_pt[pti]])
                pend.append((qc, kt, c0, n, pti))
                if len(pend) > 2:
                    issue_pv(pend.pop(0))
            while pend:
                issue_pv(pend.pop(0))

        for j in range(4):
            w = wp[j % 2]
            bw = b_wp[j % 2]
            if j + 1 < 4:
                load_pair(j + 1)
            for chunk in range(NCH):
                cs = slice(chunk * 512, (chunk + 1) * 512)
                pi = chunk % 2
                proj_fm(w, bw, 0, chunk, pi)
                P.op("scalar", lambda e, cs=cs, pi=pi: e.activation(out=qaug[0:64, 0, cs], in_=ps[pi][0:64, :],
                                                                    func=AF.Copy, scale=0.125),
                     reads=[bps[pi]], writes=[b_q[0][chunk]])
                P.op("vector", lambda e, cs=cs, pi=pi: e.tensor_scalar(out=qaug[0:64, 1, cs], in0=ps[pi][64:128, :],
                                                                       scalar1=0.125, scalar2=None, op0=ALU.mult),
                     reads=[bps[pi]], writes=[b_q[1][chunk]])
            for chunk in range(NCH):
                cs = slice(chunk * 512, (chunk + 1) * 512)
                pi = chunk % 2
                proj_fm(w, bw, 128, chunk, pi)
                P.op("scalar", lambda e, cs=cs, pi=pi: e.activation(out=kaug[0:64, 0, cs], in_=ps[pi][0:64, :],
                                                                    func=AF.Copy),
                     reads=[bps[pi]], writes=[b_k[0][chunk]])
                P.op("vector", lambda e, cs=cs, pi=pi: e.tensor_copy(out=kaug[0:64, 1, cs], in_=ps[pi][64:128, :]),
                     reads=[bps[pi]], writes=[b_k[1][chunk]])
            for chunk in range(NCH):
                cs = slice(chunk * 512, (chunk + 1) * 512)
                pi = chunk % 2
                proj_fm(w, bw, 256, chunk, pi)
                P.op("scalar", lambda e, cs=cs, pi=pi: e.activation(out=zs[:, cs], in_=ps[pi][:, :], func=AF.Silu),
                     reads=[bps[pi]], writes=[b_z[chunk]])
            for hh in range(2):
                h = 2 * j + hh
                if br == 1:
                    P.op("vector", lambda e, hh=hh: e.tensor_reduce(
                        out=km[0:64, hh * 8:(hh + 1) * 8],
                        in_=kaug[0:64, hh, :].rearrange("p (b k) -> p b k", k=256), axis=AX.X, op=ALU.add),
                        reads=b_k[hh], writes=[b_km])
                    P.op("vector", lambda e, hh=hh: e.tensor_copy(out=kmb[0:64, hh * 8:(hh + 1) * 8],
                                                                  in_=km[0:64, hh * 8:(hh + 1) * 8]),
                         reads=[b_km], writes=[b_km])
                    for t in range(NT):
                        P.op("tensor", lambda e, t=t, hh=hh: e.matmul(
                            ps[6][:, t * 8:(t + 1) * 8], lhsT=qaug[0:64, hh, t * 128:(t + 1) * 128],
                            rhs=kmb[0:64, hh * 8:(hh + 1) * 8], start=True, stop=True),
                            reads=[b_q[hh][t // 4], b_km], writes=[bps[6]])
                    P.op("vector", lambda e: e.tensor_tensor(out=gp, in0=ps[6][:, 0:128], in1=mtable, op=ALU.add),
                         reads=[bps[6]] + RC, writes=[b_sm])
                    for t in range(NT):
                        P.op("vector", lambda e, t=t: e.max(out=srt[:, t * 8:(t + 1) * 8], in_=gp[:, t * 8:(t + 1) * 8]),
                             reads=[b_sm], writes=[b_sm])
                    P.op("vector", lambda e: e.tensor_scalar(
                        out=thr, in0=srt.rearrange("p (t k) -> p t k", k=8)[:, :, 3], scalar1=-1e29, scalar2=None,
                        op0=ALU.max), reads=[b_sm], writes=[b_sm])
                    for t in range(NT):
                        P.op("vector", lambda e, t=t, h=h: e.tensor_scalar(
                            out=ext[:, t, h, 0:8], in0=gp[:, t * 8:(t + 1) * 8], scalar1=thr[:, t:t + 1],
                            scalar2=-BIG, op0=ALU.is_lt, op1=ALU.mult), reads=[b_sm], writes=[b_ext])
                for chunk in range(NCH):
                    pb = chunk % 2
                    for tt in range(4):
                        t = chunk * 4 + tt
                        P.op("tensor", lambda e, t=t, tt=tt, pb=pb, h=h: e.matmul(
                            psb[pb][0:10, tt * 128:(tt + 1) * 128], lhsT=ext[:, t, h, :], rhs=ident,
                            start=True, stop=True),
                            reads=[b_ext] + RC, writes=[bpsb[pb]])
                    P.op("vector", lambda e, chunk=chunk, pb=pb, hh=hh: e.tensor_copy(
                        out=qaug[64:74, hh, chunk * 512:(chunk + 1) * 512], in_=psb[pb][0:10, :]),
                        reads=[bpsb[pb]], writes=[b_q[hh][chunk]])
                attention(hh, h)
        P.barrier()

    def mlstm_stage(s, l):
        cur[0] = stage0
        qkT = tl(4 * S, BF16).rearrange("p (c n) -> p c n", c=4)
        cst = tl(S + 4, F32)
        acc = tl(S, F32)
        vC = tl(NT * 512, BF16).rearrange("p (t n) -> p t n", t=NT)
        oT = tl(S, BF16)
        zT = tl(S, BF16)
        faug = tl(2 * S, BF16).rearrange("p (h n) -> p h n", h=2)
        wv = tl(8 * 512, BF16).rearrange("p (c n) -> p c n", c=8)
        wf = tl(8 * 8, BF16).rearrange("p (c n) -> p c n", c=8)
        wq = [tl(8 * 128, BF16).rearrange("p (c n) -> p c n", c=8) for _ in range(2)]
        wp = [tl(8 * 256, BF16).rearrange("p (c n) -> p c n", c=8) for _ in range(2)]
        dt_ = [tl(512, F32) for _ in range(3)]
        st_ = [tl(512, BF16) for _ in range(4)]
        ext = tl(NT * 4 * 4, BF16).rearrange("p (t h e) -> p t h e", t=NT, h=4)
        u1 = tl(128, F32)
        spf = tl(64, F32)
        e1 = tl(64, F32)
        gf = tl(64, F32)
        bias_c = tl(64, F32)
        r1 = tl(64, F32)
        fa = [tl(512, F32) for _ in range(2)]
        fb = [tl(512, F32) for _ in range(2)]
        fsq = [tl(512, BF16) for _ in range(2)]
        b_qk = [[Buf("qk") for _ in range(NCH)] for _ in range(4)]
        b_cst, b_acc = Buf("cst"), Buf("acc")
        b_v = [Buf("v") for _ in range(NT)]
        b_o = [Buf("o") for _ in range(NCH)]
        b_z = [Buf("z") for _ in range(NCH)]
        b_fa = [[Buf("faug") for _ in range(NCH)] for _ in range(2)]
        b_wv, b_wf = Buf("wv"), Buf("wf")
        b_wq = [Buf("wq0"), Buf("wq1")]
        b_wp = [Buf("wp0"), Buf("wp1")]
        b_dt = [Buf("dt") for _ in range(3)]
        b_st = [Buf("st") for _ in range(4)]
        b_ext, b_sm = Buf("ext"), Buf("small")
        b_f = [[Buf("fa"), Buf("fb"), Buf("fsq")] for _ in range(2)]
        yt = yT[2]
        byt = b_yT[2]

        P.op("gpsimd", lambda e: e.memset(cst[:, 0:4], 0.0), writes=[b_cst])
        P.op("gpsimd", lambda e: e.memset(faug.rearrange("p h n -> p (h n)"), 0.0), writes=[x for y in b_fa for x in y])
        wload(wv, b_wv, "wv", l, [(OFF["c_v"], 512, 0)])
        wload(wf, b_wf, "wf", l, [(OFF["c_if"], 8, 0)])
        wload(wq[0], b_wq[0], "wq0", l, [(OFF["c_qk"], 128, 0)])

        for t in range(NT):
            pi = t % 2
            for c in range(8):
                P.op("tensor", lambda e, c=c, t=t, pi=pi: e.matmul(
                    ps[pi][:, :], lhsT=hnT[:, c, t * 128:(t + 1) * 128], rhs=wv[:, c, :],
                    start=(c == 0), stop=(c == 7)), reads=[b_wv, b_hnT[t // 4]], writes=[bps[pi]])
            eng = "scalar" if t % 2 == 0 else "vector"
            if eng == "scalar":
                P.op("scalar", lambda e, t=t, pi=pi: e.activation(out=vC[:, t, :], in_=ps[pi][:, :], func=AF.Copy),
                     reads=[bps[pi]], writes=[b_v[t]])
            else:
                P.op("vector", lambda e, t=t, pi=pi: e.tensor_copy(out=vC[:, t, :], in_=ps[pi][:, :]),
                     reads=[bps[pi]], writes=[b_v[t]])

        for t in range(NT):
            for c in range(8):
                P.op("tensor", lambda e, c=c, t=t: e.matmul(
                    ps[6][:, t * 8:(t + 1) * 8], lhsT=hnT[:, c, t * 128:(t + 1) * 128], rhs=wf[:, c, :],
                    start=(c == 0), stop=(c == 7)), reads=[b_wf, b_hnT[t // 4]], writes=[bps[6]])
        P.op("vector", lambda e: e.tensor_tensor(out=u1, in0=ps[6][:, 0:128], in1=bif[:, l, :], op=ALU.add),
             reads=[bps[6]] + RC, writes=[b_sm])
        u1v = u1.rearrange("p (t g) -> p t g", t=NT)
        e1v = e1.rearrange("p (t g) -> p t g", t=NT)
        P.op("scalar", lambda e: e.activation(out=e1v, in_=u1v[:, :, 4:8], func=AF.Exp, scale=-1.0),
             reads=[b_sm], writes=[b_sm])
        P.op("vector", lambda e: e.tensor_scalar_add(out=e1, in0=e1, scalar1=1.0), reads=[b_sm], writes=[b_sm])
        P.op("scalar", lambda e: e.activation(out=spf, in_=e1, func=AF.Ln), reads=[b_sm], writes=[b_sm])
        for t in range(NT):
            for tp in range(t + 1):
                P.op("tensor", lambda e, t=t, tp=tp: e.matmul(
                    ps[5][:, t * 4:(t + 1) * 4], lhsT=(trif if tp == t else onesf),
                    rhs=spf[:, tp * 4:(tp + 1) * 4], start=(tp == 0), stop=(tp == t)),
                    reads=[b_sm] + RC, writes=[bps[5]])
        P.op("vector", lambda e: e.tensor_copy(out=gf, in_=ps[5][:, 0:64]), reads=[bps[5]], writes=[b_sm])
        gfv = gf.rearrange("p (t h) -> p t h", t=NT)
        P.op("vector", lambda e: e.tensor_tensor(out=bias_c.rearrange("p (t h) -> p t h", t=NT), in0=u1v[:, :, 0:4],
                                                 in1=gfv, op=ALU.add), reads=[b_sm], writes=[b_sm])
        r1v = r1.rearrange("p (t h) -> p t h", t=NT)
        P.op("vector", lambda e: e.tensor_scalar(out=ext[:, :, :, 0], in0=gfv, scalar1=-1.0, scalar2=None, op0=ALU.mult),
             reads=[b_sm], writes=[b_ext])
        P.op("vector", lambda e: e.scalar_tensor_tensor(out=r1v, in0=gfv, scalar=-1.0, in1=ext[:, :, :, 0],
                                                        op0=ALU.mult, op1=ALU.subtract),
             reads=[b_sm, b_ext], writes=[b_sm])
        P.op("vector", lambda e: e.tensor_copy(out=ext[:, :, :, 1], in_=r1v), reads=[b_sm, b_ext], writes=[b_ext])
        P.op("vector", lambda e: e.tensor_tensor(out=ext[:, :, :, 2], in0=r1v, in1=ext[:, :, :, 1], op=ALU.subtract),
             reads=[b_sm, b_ext], writes=[b_ext])
        P.op("vector", lambda e: e.memset(ext[:, :, :, 3], 0.0), reads=[b_ext], writes=[b_ext])
        for h in range(4):
            for chunk in range(NCH):
                pb = chunk % 2
                for tt in range(4):
                    t = chunk * 4 + tt
                    P.op("tensor", lambda e, t=t, tt=tt, pb=pb, h=h: e.matmul(
                        psb[pb][0:4, tt * 128:(tt + 1) * 128], lhsT=ext[:, t, h, :], rhs=ident,
                        start=True, stop=True),
                        reads=[b_ext] + RC, writes=[bpsb[pb]])
                r0 = 32 * (h % 2)
                P.op("vector", lambda e, chunk=chunk, pb=pb, h=h, r0=r0: e.tensor_copy(
                    out=faug[r0:r0 + 4, h // 2, chunk * 512:(chunk + 1) * 512], in_=psb[pb][0:4, :]),
                    reads=[bpsb[pb]], writes=[b_fa[h // 2][chunk]])

        for cc in range(4):
            w = wq[cc % 2]
            bw = b_wq[cc % 2]
            if cc + 1 < 4:
                wload(wq[(cc + 1) % 2], b_wq[(cc + 1) % 2], "wq%d" % ((cc + 1) % 2), l,
                      [(OFF["c_qk"] + (cc + 1) * 128, 128, 0)])
            for chunk in range(NCH):
                pi = chunk % 2
                proj_fm(w, bw, 0, chunk, pi)
                eng = "scalar" if chunk % 2 == 0 else "vector"
                dst = cst[:, 4 + chunk * 512:4 + (chunk + 1) * 512]
                if eng == "scalar":
                    P.op("scalar", lambda e, dst=dst, pi=pi: e.activation(out=dst, in_=ps[pi][:, :], func=AF.Copy),
                         reads=[bps[pi]], writes=[b_cst])
                else:
                    P.op("vector", lambda e, dst=dst, pi=pi: e.tensor_copy(out=dst, in_=ps[pi][:, :]),
                         reads=[bps[pi]], writes=[b_cst])
            P.op("gpsimd", lambda e, cc=cc: e.tensor_scalar(out=acc, in0=cst[:, 4:4 + S], scalar1=cw[:, l, cc, 3:4],
                                                            scalar2=None, op0=ALU.mult),
                 reads=[b_cst] + RC, writes=[b_acc])
            for jj in (2, 1, 0):
                sh = 3 - jj
                P.op("vector", lambda e, cc=cc, jj=jj, sh=sh: e.scalar_tensor_tensor(
                    out=acc, in0=cst[:, 4 - sh:4 - sh + S], scalar=cw[:, l, cc, jj:jj + 1], in1=acc,
                    op0=ALU.mult, op1=ALU.add), reads=[b_cst, b_acc] + RC, writes=[b_acc])
            for chunk in range(NCH):
                P.op("scalar", lambda e, cc=cc, chunk=chunk: e.activation(
                    out=qkT[:, cc, chunk * 512:(chunk + 1) * 512], in_=acc[:, chunk * 512:(chunk + 1) * 512],
                    func=AF.Silu), reads=[b_acc], writes=[b_qk[cc][chunk]])

        def load_head(h):
            wload(wp[h % 2], b_wp[h % 2], "wp%d" % (h % 2), l,
                  [(OFF["c_o"] + h * 128, 128, 0), (OFF["c_z"] + h * 128, 128, 128)])
        load_head(0)

        def attention(h):
            pb0 = 64 * (h % 2)
            qch = h // 2
            kch = 2 + h // 2
            r0 = 32 * (h % 2)
            fs = h // 2
            tiles = [(qc, kt) for qc in range(NCH) for kt in range(4 * qc + 4)]
            pend = []
            cnt = [0]

            def issue_pv(item):
                qc, kt, c0, n, sti = item
                P.op("tensor", lambda e: e.matmul(ps[4][:, c0:c0 + n], lhsT=vC[:, kt, h * 128:(h + 1) * 128],
                                                  rhs=st_[sti][:, 0:n], start=(kt == 0), stop=(kt == 4 * qc + 3)),
                     reads=[b_v[kt], b_st[sti]], writes=[bps[4]])
                P.op("tensor", lambda e: e.matmul(ps[5][:, c0:c0 + n], lhsT=onesb, rhs=st_[sti][:, 0:n],
                                                  start=(kt == 0), stop=(kt == 4 * qc + 3)),
                     reads=[b_st[sti]] + RC, writes=[bps[5]])
                if kt == 4 * qc + 3:
                    fin(qc)

            def fin(qc):
                fi = qc % 2
                cs = slice(qc * 512, (qc + 1) * 512)
                bf_a, bf_b, bf_s = b_f[fi]
                P.op("scalar", lambda e: e.activation(out=fa[fi], in_=ps[5][:, :], func=AF.Abs),
                     reads=[bps[5]], writes=[bf_a])
                P.op("vector", lambda e: e.tensor_scalar_max(out=fa[fi], in0=fa[fi], scalar1=1.0),
                     reads=[bf_a], writes=[bf_a])
                P.op("vector", lambda e: e.reciprocal(out=fa[fi], in_=fa[fi]), reads=[bf_a], writes=[bf_a])
                P.op("vector", lambda e: e.tensor_tensor(out=fb[fi], in0=ps[4][:, :], in1=fa[fi], op=ALU.mult),
                     reads=[bps[4], bf_a], writes=[bf_b])
                P.op("gpsimd", lambda e: e.tensor_tensor(out=fb[fi], in0=fb[fi], in1=oT[:, cs], op=ALU.mult),
                     reads=[bf_b, b_o[qc]], writes=[bf_b])
                P.op("scalar", lambda e: e.activation(out=fsq[fi], in_=fb[fi], func=AF.Square),
                     reads=[bf_b], writes=[bf_s])
                P.op("tensor", lambda e: e.matmul(ps[6][:, :], lhsT=o128b, rhs=fsq[fi], start=True, stop=True),
                     reads=[bf_s] + RC, writes=[bps[6]])
                P.op("scalar", lambda e: e.activation(out=fa[fi], in_=ps[6][:, :], func=AF.Ln, bias=epsc),
                     reads=[bps[6]] + RC, writes=[bf_a])
                P.op("scalar", lambda e: e.activation(out=fa[fi], in_=fa[fi], func=AF.Exp, scale=-0.5),
                     reads=[bf_a], writes=[bf_a])
                P.op("vector", lambda e: e.tensor_tensor(out=fb[fi], in0=fb[fi], in1=fa[fi], op=ALU.mult),
                     reads=[bf_a, bf_b], writes=[bf_b])
                P.op("vector", lambda e: e.scalar_tensor_tensor(out=yt[:, h, cs], in0=fb[fi], scalar=hg[:, l, h:h + 1],
                                                                in1=zT[:, cs], op0=ALU.mult, op1=ALU.mult),
                     reads=[bf_b, b_z[qc]] + RC, writes=[byt[h][qc]])

            for (qc, kt) in tiles:
                d = kt - 4 * qc
                c0 = 0 if d < 0 else d * 128
                n = 512 - c0
                k_ = cnt[0]
                cnt[0] += 1
                si = 2 + (k_ % 2)
                ei = k_ % 2
                di = k_ % 3
                sti = k_ % 4
                q0 = qc * 512 + c0
                P.op("tensor", lambda e, kt=kt, q0=q0, n=n, si=si: e.matmul(
                    ps[si][:, 0:n], lhsT=qkT[pb0:pb0 + 64, kch, kt * 128:(kt + 1) * 128],
                    rhs=qkT[pb0:pb0 + 64, qch, q0:q0 + n], start=True, stop=True),
                    reads=[b_qk[kch][kt // 4], b_qk[qch][qc]], writes=[bps[si]])
                P.op("tensor", lambda e, q0=q0, n=n, ei=ei: e.matmul(
                    ps[ei][:, 0:n], lhsT=onesb[r0:r0 + 4, :], rhs=faug[r0:r0 + 4, fs, q0:q0 + n],
                    start=True, stop=True), reads=[b_fa[fs][qc]] + RC, writes=[bps[ei]])
                P.op("scalar", lambda e, kt=kt, n=n, ei=ei, di=di: e.activation(
                    out=dt_[di][:, 0:n], in_=ps[ei][:, 0:n], func=AF.Exp, bias=bias_c[:, kt * 4 + h:kt * 4 + h + 1]),
                    reads=[bps[ei], b_sm], writes=[b_dt[di]])
                P.op("vector", lambda e, n=n, si=si, di=di, sti=sti: e.scalar_tensor_tensor(
                    out=st_[sti][:, 0:n], in0=ps[si][:, 0:n], scalar=0.125, in1=dt_[di][:, 0:n],
                    op0=ALU.mult, op1=ALU.mult), reads=[bps[si], b_dt[di]], writes=[b_st[sti]])
                if d >= 0:
                    P.op("gpsimd", lambda e, sti=sti: e.tensor_tensor(out=st_[sti][:, 0:128], in0=st_[sti][:, 0:128],
                                                                      in1=tri01, op=ALU.mult),
                         reads=[b_st[sti]] + RC, writes=[b_st[sti]])
                pend.append((qc, kt, c0, n, sti))
                if len(pend) > 2:
                    issue_pv(pend.pop(0))
            while pend:
                issue_pv(pend.pop(0))

        for h in range(4):
            w = wp[h % 2]
            bw = b_wp[h % 2]
            if h + 1 < 4:
                load_head(h + 1)
            for chunk in range(NCH):
                cs = slice(chunk * 512, (chunk + 1) * 512)
                pi = chunk % 2
                proj_fm(w, bw, 0, chunk, pi)
                P.op("scalar", lambda e, cs=cs, pi=pi: e.activation(out=oT[:, cs], in_=ps[pi][:, :], func=AF.Sigmoid),
                     reads=[bps[pi]], writes=[b_o[chunk]])
            for chunk in range(NCH):
                cs = slice(chunk * 512, (chunk + 1) * 512)
                pi = chunk % 2
                proj_fm(w, bw, 128, chunk, pi)
                P.op("scalar", lambda e, cs=cs, pi=pi: e.activation(out=zT[:, cs], in_=ps[pi][:, :], func=AF.Silu),
                     reads=[bps[pi]], writes=[b_z[chunk]])
            attention(h)
        P.barrier()

    def merge_stage(s, l, finals):
        cur[0] = stage0
        wo = tl(8 * D, BF16).rearrange("p (c n) -> p c n", c=8)
        wb = tl(12 * D, BF16).rearrange("p (c n) -> p c n", c=12)
        wg = [tl(8 * 128, BF16).rearrange("p (c n) -> p c n", c=8) for _ in range(2)]
        mbf = tl(8 * 1024, BF16).rearrange("p (c n) -> p c n", c=8)
        macc = [tl(512, F32) for _ in range(2)]
        gs = [tl(512, F32) for _ in range(2)]
        tmp = [tl(512, F32) for _ in range(2)]
        xr = [tl(D, F32) for _ in range(2)]
        xn = [tl(D, F32) for _ in range(2)]
        jk = tl(D, BF16)
        ss = tl(NT, F32)
        b_wo, b_wb = Buf("wo"), Buf("wb")
        b_wg = [Buf("wg0"), Buf("wg1")]
        b_mbf = [Buf("mbf") for _ in range(8)]
        b_macc = [Buf("macc0"), Buf("macc1")]
        b_gs = [Buf("gs0"), Buf("gs1")]
        b_tmp = [Buf("tmp0"), Buf("tmp1")]
        b_xr = [Buf("xr0"), Buf("xr1")]
        b_xn = [Buf("xn0"), Buf("xn1")]
        b_jk, b_ss = Buf("jk"), Buf("ss")
        src = x_in[s] if l == 0 else xs_d[s]

        P.op("gpsimd", lambda e: e.dma_start(out=wo, in_=w_out[l].rearrange("(c p) n -> p c n", p=128)),
             writes=[b_wo], dma_ch="wo")
        P.op("gpsimd", lambda e: [e.dma_start(out=wb[:, n * 4:(n + 1) * 4, :],
                                            in_=w_br[l, n].rearrange("(c p) d -> p c d", p=128)) for n in range(3)],
             writes=[b_wb], dma_ch="wb", dma_n=3)
        gcnt = [0]
        for half in range(2):
            for ft in range(8):
                for n in range(3):
                    gi = gcnt[0] % 2
                    gcnt[0] += 1
                    wload(wg[gi], b_wg[gi], "wg%d" % gi, l, [(OFF["gates"] + n * 1024 + ft * 128, 128, 0)])
                    for cc in range(2):
                        chunk = half * 2 + cc
                        cs = slice(chunk * 512, (chunk + 1) * 512)
                        pg = cc
                        pbk = 2 + cc
                        for c in range(8):
                            P.op("tensor", lambda e, c=c, gi=gi, cs=cs, pg=pg: e.matmul(
                                ps[pg][:, :], lhsT=wg[gi][:, c, :], rhs=hnT[:, c, cs], start=(c == 0), stop=(c == 7)),
                                reads=[b_wg[gi], b_hnT[chunk]], writes=[bps[pg]])
                        for wc in range(4):
                            P.op("tensor", lambda e, wc=wc, n=n, ft=ft, cs=cs, pbk=pbk: e.matmul(
                                ps[pbk][:, :], lhsT=wb[:, n * 4 + wc, ft * 128:(ft + 1) * 128], rhs=yT[n][:, wc, cs],
                                start=(wc == 0), stop=(wc == 3)),
                                reads=[b_wb, b_yT[n][wc][chunk]], writes=[bps[pbk]])
                        P.op("scalar", lambda e, cc=cc, pg=pg: e.activation(out=gs[cc], in_=ps[pg][:, :], func=AF.Sigmoid),
                             reads=[bps[pg]], writes=[b_gs[cc]])
                        if n == 0:
                            P.op("vector", lambda e, cc=cc, pbk=pbk: e.tensor_tensor(out=macc[cc], in0=ps[pbk][:, :],
                                                                                     in1=gs[cc], op=ALU.mult),
                                 reads=[bps[pbk], b_gs[cc]], writes=[b_macc[cc]])
                        else:
                            P.op("vector", lambda e, cc=cc, pbk=pbk: e.tensor_tensor(out=tmp[cc], in0=ps[pbk][:, :],
                                                                                     in1=gs[cc], op=ALU.mult),
                                 reads=[bps[pbk], b_gs[cc]], writes=[b_tmp[cc]])
                            if n == 1:
                                P.op("gpsimd", lambda e, cc=cc: e.tensor_tensor(out=macc[cc], in0=macc[cc], in1=tmp[cc],
                                                                                op=ALU.add),
                                     reads=[b_tmp[cc], b_macc[cc]], writes=[b_macc[cc]])
                            else:
                                P.op("gpsimd", lambda e, cc=cc, ft=ft: e.tensor_tensor(
                                    out=mbf[:, ft, cc * 512:(cc + 1) * 512], in0=macc[cc], in1=tmp[cc], op=ALU.add),
                                    reads=[b_tmp[cc], b_macc[cc]], writes=[b_mbf[ft]])
            for tt in range(8):
                t = half * 8 + tt
                i = tt % 2
                P.op("sync", lambda e, i=i, t=t: e.dma_start(out=xr[i], in_=src[t * 128:(t + 1) * 128, :]),
                     writes=[b_xr[i]], dma_ch="xr%d" % i)
                for hc in range(2):
                    po = 4 + hc
                    for fc in range(8):
                        P.op("tensor", lambda e, fc=fc, tt=tt, hc=hc, po=po: e.matmul(
                            ps[po][:, :], lhsT=mbf[:, fc, tt * 128:(tt + 1) * 128], rhs=wo[:, fc, hc * 512:(hc + 1) * 512],
                            start=(fc == 0), stop=(fc == 7)), reads=[b_wo, b_mbf[fc]], writes=[bps[po]])
                    P.op("vector", lambda e, i=i, hc=hc, po=po: e.tensor_tensor(
                        out=xn[i][:, hc * 512:(hc + 1) * 512], in0=ps[po][:, :], in1=xr[i][:, hc * 512:(hc + 1) * 512],
                        op=ALU.add), reads=[bps[po], b_xr[i]], writes=[b_xn[i]])
                if l == 0:
                    P.op("sync", lambda e, i=i, t=t: e.dma_start(out=xs_d[s, t * 128:(t + 1) * 128, :], in_=xn[i]),
                         reads=[b_xn[i]], dma_ch="st%d" % i)
                    if dbg and s == 0:
                        finals.append(P.op("sync", lambda e, i=i, t=t: e.dma_start(
                            out=dbg_d["dbg_x1"][t * 128:(t + 1) * 128, :], in_=xn[i]), reads=[b_xn[i]], dma_ch="dbgx"))
                else:
                    P.op("scalar", lambda e, i=i, t=t: e.activation(out=jk, in_=xn[i], func=AF.Square,
                                                                    accum_out=ss[:, t:t + 1]),
                         reads=[b_xn[i]], writes=[b_jk, b_ss])
                    P.op("scalar", lambda e, t=t: e.activation(out=ss[:, t:t + 1], in_=ss[:, t:t + 1], func=AF.Ln,
                                                               bias=epsc, scale=1.0 / D),
                         reads=[b_ss] + RC, writes=[b_ss])
                    P.op("scalar", lambda e, t=t: e.activation(out=ss[:, t:t + 1], in_=ss[:, t:t + 1], func=AF.Exp,
                                                               scale=-0.5),
                         reads=[b_ss], writes=[b_ss])
                    P.op("vector", lambda e, i=i, t=t: e.scalar_tensor_tensor(
                        out=xn[i], in0=xn[i], scalar=ss[:, t:t + 1], in1=fgrep, op0=ALU.mult, op1=ALU.mult),
                        reads=[b_xn[i], b_ss] + RC, writes=[b_xn[i]])
                    finals.append(P.op("sync", lambda e, i=i, t=t: e.dma_start(
                        out=out_d[s, t * 128:(t + 1) * 128, :], in_=xn[i]), reads=[b_xn[i]], dma_ch="st%d" % i))
        P.barrier()

    def dump(nm, src_ap, bufs, finals):
        finals.append(P.op("gpsimd", lambda e: e.dma_start(out=dbg_d[nm], in_=src_ap), reads=bufs, dma_ch="dbg_" + nm))

    finals = []
    import os
    limit = int(os.environ.get("MK_LIMIT", "1000"))
    nst = [0]

    def go(fn, *a):
        if nst[0] < limit:
            fn(*a)
        nst[0] += 1

    for s in range(NSEQ):
        for l in range(DEPTH):
            go(norm_stage, s, l)
            if dbg and s == 0 and l == 0:
                dump("dbg_hnT", hnT.rearrange("p c n -> p (c n)"), b_hnT, finals)
                P.barrier()
            go(attn_stage, s, l, 0)
            go(attn_stage, s, l, 1)
            go(mlstm_stage, s, l)
            if dbg and s == 0 and l == 0:
                for bi, nm in enumerate(("dbg_ya", "dbg_yb", "dbg_yc")):
                    dump(nm, yT[bi].rearrange("p c n -> p (c n)"), [x for y in b_yT[bi] for x in y], finals)
                P.barrier()
            go(merge_stage, s, l, finals)
            P.barrier(new_epoch=True)
    P.emit(final_waits=finals)
    return nc


_CACHE = {}


def prep_inputs(inputs):
    f = np.float32
    norm_g = np.asarray(inputs["norm_g"], f)
    gfull = np.zeros((128, DEPTH, 8, 128), f)
    for l in range(DEPTH):
        gfull[:, l, :, :] = norm_g[l].reshape(8, 128).T[:, :, None]
    shared = {
        "w_in": np.ascontiguousarray(inputs["w_in"], f),
        "w_branch": np.ascontiguousarray(inputs["w_branch"], f),
        "w_out": np.ascontiguousarray(inputs["w_out"], f),
        "gfull": gfull.reshape(128, -1),
        "fgrep": np.ascontiguousarray(np.broadcast_to(np.asarray(inputs["final_norm_g"], f)[None, :], (128, D))),
    }
    fbf = np.zeros((128, DEPTH, NT, 8), f)
    bif = np.zeros((128, DEPTH, NT, 8), f)
    for l in range(DEPTH):
        fbf[:, l, :, :] = np.asarray(inputs["fox_b_f"], f)[l][None, None, :]
        bif[:, l, :, 0:4] = np.asarray(inputs["mlstm_b_i"], f)[l][None, None, :]
        bif[:, l, :, 4:8] = np.asarray(inputs["mlstm_b_f"], f)[l][None, None, :]
    shared["fbf_rep"] = fbf.reshape(128, -1)
    shared["bif_rep"] = bif.reshape(128, -1)
    cwv = np.asarray(inputs["mlstm_conv_w"], f)
    cw = np.zeros((128, DEPTH, 4, 4), f)
    for l in range(DEPTH):
        cw[:, l, :, :] = cwv[l].reshape(4, 4, 128).transpose(2, 1, 0)
    shared["cw"] = cw.reshape(128, -1)
    hgv = np.asarray(inputs["mlstm_head_g"], f)
    hg = np.zeros((128, DEPTH, 4), f)
    for l in range(DEPTH):
        hg[:, l, :] = hgv[l].reshape(4, 128).T
    shared["hg"] = hg.reshape(128, -1)
    for k, v in host_consts().items():
        shared["c_" + k] = np.ascontiguousarray(v, f)
    return shared


def kernel(**inputs):
    x = np.ascontiguousarray(inputs["x"], np.float32)
    shared = prep_inputs(inputs)
    if "nc" not in _CACHE:
        _CACHE["nc"] = build(False)
    nc = _CACHE["nc"]
    in_maps = []
    for c in range(8):
        m = dict(shared)
        m["x"] = np.ascontiguousarray(x[2 * c:2 * c + 2])
        in_maps.append(m)
    res = run_bass_kernel_spmd(nc, in_maps, core_ids=list(range(8)))
    out = np.concatenate([np.asarray(r["out"], np.float32) for r in res.results], axis=0)
    return out
```

```python
import contextlib
import numpy as np
import concourse.bass as bass
import concourse.mybir as mybir
from concourse.bass_utils import run_bass_kernel_spmd

F32 = mybir.dt.float32
BF16 = mybir.dt.bfloat16
U8 = mybir.dt.uint8
AF = mybir.ActivationFunctionType
ALU = mybir.AluOpType
AX = mybir.AxisListType

S = 2048
D = 1024
NT = 16
NCH = 4
DEPTH = 2
NSEQ = 2
INW = 9232
OFF = dict(a_q=0, a_k=512, a_v=1024, a_z=1536, a_f=2048, b_q=2056, b_k=2568, b_v=3080,
           b_z=3592, c_qk=4104, c_v=4616, c_o=5128, c_z=5640, c_if=6152, gates=6160)
EPS = 1e-6
BIG = 30000.0


class Buf:
    __slots__ = ("name", "last_w", "reads")

    def __init__(self, name):
        self.name = name
        self.last_w = None
        self.reads = []


class Op:
    __slots__ = ("eng", "fn", "deps", "signal", "sigval", "dma_ch", "dma_n", "epoch", "idx")


class Prog:
    ENGS = ("tensor", "vector", "scalar", "gpsimd", "sync")

    def __init__(self, nc):
        self.nc = nc
        self.q = {e: [] for e in self.ENGS}
        self.ops = []
        self.epoch = 0
        self.last_real = {e: None for e in self.ENGS}
        self.last_dma = {}
        self.channels = []

    def op(self, eng, fn, reads=(), writes=(), dma_ch=None, dma_n=1, extra=()):
        o = Op()
        o.eng = eng
        o.fn = fn
        o.signal = False
        o.sigval = None
        o.dma_ch = dma_ch
        o.dma_n = dma_n
        o.epoch = self.epoch
        o.idx = len(self.ops)
        deps = set(extra)
        for b in reads:
            if b.last_w is not None:
                deps.add(b.last_w)
        for b in writes:
            if b.last_w is not None:
                deps.add(b.last_w)
            deps.update(b.reads)
        deps.discard(o)
        best = {}
        for d in deps:
            if d.fn is None:
                continue
            if d.dma_ch is not None:
                key = ("dma", d.dma_ch)
            else:
                if d.eng == "tensor" and eng == "tensor" and dma_ch is None:
                    continue
                key = ("eng", d.eng, d.epoch)
            if key not in best or best[key].idx < d.idx:
                best[key] = d
        o.deps = list(best.values())
        for b in reads:
            b.reads.append(o)
        for b in writes:
            b.last_w = o
            b.reads = []
        self.q[eng].append(o)
        self.ops.append(o)
        if fn is not None:
            if dma_ch is not None:
                self.last_dma[dma_ch] = o
                if dma_ch not in self.channels:
                    self.channels.append(dma_ch)
            else:
                self.last_real[eng] = o
        return o

    def barrier(self, new_epoch=False):
        tgt = [o for o in self.last_real.values() if o is not None] + list(self.last_dma.values())
        for e in self.ENGS:
            self.op(e, None, extra=tgt)
        if new_epoch:
            self.epoch += 1

    def emit(self, final_waits=()):
        nc = self.nc
        for o in self.ops:
            for d in o.deps:
                d.signal = True
        for o in final_waits:
            o.signal = True
        with contextlib.ExitStack() as st:
            esem = {}
            for ep in range(self.epoch + 1):
                for e in self.ENGS:
                    esem[(ep, e)] = st.enter_context(nc.semaphore("s%d_%s" % (ep, e)))
            dsem = {c: st.enter_context(nc.semaphore("d_" + c)) for c in self.channels}
            cnt = {}
            chcnt = {}
            for e in self.ENGS:
                for o in self.q[e]:
                    if o.fn is None:
                        continue
                    if o.dma_ch is not None:
                        chcnt[o.dma_ch] = chcnt.get(o.dma_ch, 0) + 16 * o.dma_n
                        o.sigval = (dsem[o.dma_ch], chcnt[o.dma_ch])
                    elif o.signal:
                        k = (o.epoch, e)
                        cnt[k] = cnt.get(k, 0) + 1
                        o.sigval = (esem[k], cnt[k])
            prog = self

            def run(e, engine):
                seen = {}
                for o in prog.q[e]:
                    need = {}
                    for d in o.deps:
                        if d.sigval is None:
                            continue
                        s, v = d.sigval
                        k = id(s)
                        if seen.get(k, 0) >= v:
                            continue
                        if k not in need or need[k][1] < v:
                            need[k] = (s, v)
                    for k, (s, v) in need.items():
                        engine.wait_ge(s, v)
                        seen[k] = v
                    if o.fn is None:
                        continue
                    ins = o.fn(engine)
                    if o.dma_ch is not None:
                        lst = ins if isinstance(ins, (list, tuple)) else [ins]
                        assert len(lst) == o.dma_n, (len(lst), o.dma_n)
                        for i_ in lst:
                            i_.then_inc(o.sigval[0], 16)
                    elif o.signal:
                        ins.then_inc(o.sigval[0], 1)
                if e == "sync":
                    for o in final_waits:
                        s, v = o.sigval
                        engine.wait_ge(s, v)

            with nc.Block() as block:
                @block.tensor
                def _(eng):
                    run("tensor", eng)

                @block.vector
                def _(eng):
                    run("vector", eng)

                @block.scalar
                def _(eng):
                    run("scalar", eng)

                @block.gpsimd
                def _(eng):
                    run("gpsimd", eng)

                @block.sync
                def _(eng):
                    run("sync", eng)


def host_consts():
    c = {}
    c["ident"] = np.eye(128, dtype=np.float32)
    kk = np.arange(128)
    c["tri01"] = (kk[None, :] >= kk[:, None]).astype(np.float32)
    c["trif"] = (kk[:, None] <= kk[None, :]).astype(np.float32)
    pos = np.arange(S)
    ksd = np.zeros((10, 2, S), np.float32)
    for kb in range(8):
        ksd[kb, :, :] = (pos // 256 == kb).astype(np.float32)[None, :]
    ksd[8:10] = 1.0
    c["kside"] = ksd
    mt = np.zeros((128, NT, 8), np.float32)
    for t in range(NT):
        own = t // 2
        for kb in range(8):
            mt[:, t, kb] = 0.0 if kb < own else (1e30 if kb == own else -1e30)
    c["mtable"] = mt.reshape(128, NT * 8)
    slopes = 2.0 ** (-8.0 * (np.arange(8) + 1.0) / 8)
    tokpos = (np.arange(NT)[None, :] * 128 + np.arange(128)[:, None]).astype(np.float64)
    hi = np.floor(tokpos / 256) * 256
    lo = tokpos - hi
    al = np.zeros((128, NT, 8, 2), np.float32)
    kb_ = np.zeros((128, NT, 8), np.float32)
    for h in range(8):
        al[:, :, h, 0] = -slopes[h] * hi
        al[:, :, h, 1] = -slopes[h] * lo
        kb_[:, :, h] = slopes[h] * tokpos
    c["alibi_q"] = al.reshape(128, NT * 8 * 2)
    c["alibi_k"] = kb_.reshape(128, NT * 8)
    return c


CONST_SHAPES = dict(ident=(128, 128), tri01=(128, 128), trif=(128, 128), kside=(10, 2, S),
                    mtable=(128, 128), alibi_q=(128, 256), alibi_k=(128, 128))


def build(dbg=False):
    nc = bass.Bass("TRN2", target_bir_lowering=False)

    def din(name, shape):
        return nc.dram_tensor(name, list(shape), F32, kind="ExternalInput").ap()

    x_in = din("x", (NSEQ, S, D))
    w_in = din("w_in", (DEPTH, D, INW))
    w_br = din("w_branch", (DEPTH, 3, 512, D))
    w_out = din("w_out", (DEPTH, D, D))
    gfull_d = din("gfull", (128, DEPTH * 8 * 128))
    fgrep_d = din("fgrep", (128, D))
    fbf_d = din("fbf_rep", (128, DEPTH * 128))
    bif_d = din("bif_rep", (128, DEPTH * 128))
    cw_d = din("cw", (128, DEPTH * 16))
    hg_d = din("hg", (128, DEPTH * 4))
    cd = {k: din("c_" + k, v) for k, v in CONST_SHAPES.items()}
    out_d = nc.dram_tensor("out", [NSEQ, S, D], F32, kind="ExternalOutput").ap()
    xs_d = nc.dram_tensor("xs_scratch", [NSEQ, S, D], F32, kind="Internal").ap()
    dbg_d = {}
    if dbg:
        for nm in ("dbg_hnT",):
            dbg_d[nm] = nc.dram_tensor(nm, [128, 8 * S], F32, kind="ExternalOutput").ap()
        for nm in ("dbg_ya", "dbg_yb", "dbg_yc"):
            dbg_d[nm] = nc.dram_tensor(nm, [128, 4 * S], F32, kind="ExternalOutput").ap()
        dbg_d["dbg_x1"] = nc.dram_tensor("dbg_x1", [S, D], F32, kind="ExternalOutput").ap()

    TOTAL = 212800
    big = nc.alloc_sbuf_tensor("big", [128, TOTAL], U8)
    cur = [0]

    def alloc(nbytes):
        o = cur[0]
        cur[0] = o + ((nbytes + 63) // 64) * 64
        assert cur[0] <= TOTAL, cur[0]
        return o

    def view(off, n, dt):
        sz = 4 if dt == F32 else 2
        return big[:, off:off + n * sz].bitcast(dt)

    def tl(n, dt):
        sz = 4 if dt == F32 else 2
        return view(alloc(n * sz), n, dt)

    hnT = tl(8 * S, BF16).rearrange("p (c n) -> p c n", c=8)
    yT = [tl(4 * S, BF16).rearrange("p (c n) -> p c n", c=4) for _ in range(3)]
    ident = tl(128, BF16)
    tri01 = tl(128, BF16)
    onesb = tl(128, BF16)
    o128b = tl(128, BF16)
    trif = tl(128, F32)
    onesf = tl(128, F32)
    gfull = tl(DEPTH * 8 * 128, BF16).rearrange("p (l c j) -> p l c j", l=DEPTH, c=8)
    fgrep = tl(D, F32)
    fbf = tl(DEPTH * 128, F32).rearrange("p (l n) -> p l n", l=DEPTH)
    bif = tl(DEPTH * 128, F32).rearrange("p (l n) -> p l n", l=DEPTH)
    cw = tl(DEPTH * 16, F32).rearrange("p (l c j) -> p l c j", l=DEPTH, c=4)
    hg = tl(DEPTH * 4, F32).rearrange("p (l h) -> p l h", l=DEPTH)
    mtable = tl(128, F32)
    alibi_k = tl(128, F32)
    epsc = tl(1, F32)
    stage0 = cur[0]

    ps = [nc.alloc_psum_tensor("ps%d" % i, [128, 512], F32)[:, :] for i in range(8)]
    bps = [Buf("ps%d" % i) for i in range(8)]
    psb = [ps[7], ps[6]]
    bpsb = [bps[7], bps[6]]

    P = Prog(nc)
    b_hnT = [Buf("hnT%d" % i) for i in range(NCH)]
    b_yT = [[[Buf("y") for _ in range(NCH)] for _ in range(4)] for _ in range(3)]
    b_const = Buf("const")

    def ld(dst, src):
        q = "sync" if dst.dtype == src.dtype else "gpsimd"
        P.op(q, lambda e: e.dma_start(out=dst, in_=src), writes=[b_const], dma_ch="const_" + q)

    ld(ident, cd["ident"])
    ld(tri01, cd["tri01"])
    ld(trif, cd["trif"])
    ld(gfull.rearrange("p l c j -> p (l c j)"), gfull_d)
    ld(fgrep, fgrep_d)
    ld(fbf.rearrange("p l n -> p (l n)"), fbf_d)
    ld(bif.rearrange("p l n -> p (l n)"), bif_d)
    ld(cw.rearrange("p l c j -> p (l c j)"), cw_d)
    ld(hg.rearrange("p l h -> p (l h)"), hg_d)
    ld(mtable, cd["mtable"])
    ld(alibi_k, cd["alibi_k"])
    P.op("gpsimd", lambda e: e.memset(onesb, 1.0), writes=[b_const])
    P.op("gpsimd", lambda e: e.memset(o128b, 1.0 / 128.0), writes=[b_const])
    P.op("gpsimd", lambda e: e.memset(onesf, 1.0), writes=[b_const])
    P.op("gpsimd", lambda e: e.memset(epsc, EPS), writes=[b_const])
    P.barrier()

    RC = [b_const]
    psrr = [0]

    def proj_fm(wt, bw, j0, chunk, pidx):
        for c in range(8):
            P.op("tensor", lambda e, c=c: e.matmul(ps[pidx][:, :], lhsT=wt[:, c, j0:j0 + 128],
                                                   rhs=hnT[:, c, chunk * 512:(chunk + 1) * 512],
                                                   start=(c == 0), stop=(c == 7)),
                 reads=[bw, b_hnT[chunk]], writes=[bps[pidx]])

    def wload(wt, bw, ch, l, cols, eng="gpsimd"):
        src = w_in[l].rearrange("(c p) n -> p c n", p=128)

        def fn(e):
            return [e.dma_start(out=wt[:, :, d0:d0 + n], in_=src[:, :, c0:c0 + n]) for (c0, n, d0) in cols]
        P.op(eng, fn, writes=[bw], dma_ch=ch, dma_n=len(cols))

    def norm_stage(s, l):
        cur[0] = stage0
        xt = [tl(D, F32) for _ in range(2)]
        hnb = [tl(D, BF16) for _ in range(2)]
        ss = tl(NT, F32)
        rstd = tl(NT, F32)
        bxt = [Buf("xt0"), Buf("xt1")]
        bhnb = [Buf("hnb0"), Buf("hnb1")]
        bss = Buf("ss")
        src = x_in[s] if l == 0 else xs_d[s]
        import os
        ncut = int(os.environ.get("MK_NCUT", "99"))
        for t in range(NT):
            i = t % 2
            P.op("sync", lambda e, i=i, t=t: e.dma_start(out=xt[i], in_=src[t * 128:(t + 1) * 128, :]),
                 writes=[bxt[i]], dma_ch="xt%d" % i)
            if ncut < 1:
                continue
            P.op("scalar", lambda e, i=i, t=t: e.activation(out=hnb[i], in_=xt[i], func=AF.Square,
                                                            accum_out=ss[:, t:t + 1]),
                 reads=[bxt[i]], writes=[bhnb[i], bss])
            if ncut < 2:
                continue
            P.op("scalar", lambda e, t=t: e.activation(out=rstd[:, t:t + 1], in_=ss[:, t:t + 1], func=AF.Ln,
                                                       bias=epsc, scale=1.0 / D),
                 reads=[bss] + RC, writes=[bss])
            P.op("scalar", lambda e, t=t: e.activation(out=rstd[:, t:t + 1], in_=rstd[:, t:t + 1], func=AF.Exp,
                                                       scale=-0.5),
                 reads=[bss], writes=[bss])
            if ncut < 3:
                continue
            P.op("scalar", lambda e, i=i, t=t: e.activation(out=hnb[i], in_=xt[i], func=AF.Copy,
                                                            scale=rstd[:, t:t + 1]),
                 reads=[bxt[i], bss], writes=[bhnb[i]])
            if ncut < 4:
                continue
            for hf in range(2):
                for cc in range(4):
                    c = hf * 4 + cc
                    P.op("tensor", lambda e, i=i, c=c, cc=cc, hf=hf: e.matmul(
                        psb[hf][:, cc * 128:(cc + 1) * 128], lhsT=hnb[i][:, c * 128:(c + 1) * 128], rhs=ident,
                        start=True, stop=True),
                        reads=[bhnb[i]] + RC, writes=[bpsb[hf]])
                eng = "vector"
                if ncut < 5:
                    continue
                P.op(eng, lambda e, hf=hf, t=t: e.tensor_tensor(
                    out=hnT[:, hf * 4:(hf + 1) * 4, t * 128:(t + 1) * 128],
                    in0=psb[hf].rearrange("p (c j) -> p c j", c=4),
                    in1=gfull[:, l, hf * 4:(hf + 1) * 4, :], op=ALU.mult),
                    reads=[bpsb[hf]] + RC, writes=[b_hnT[t // 4]])
        P.barrier()

    def attn_stage(s, l, br):
        cur[0] = stage0
        pre = "a_" if br == 0 else "b_"
        qaug = tl(2 * S, BF16).rearrange("p (h n) -> p h n", h=2)
        kaug = tl(2 * S, BF16).rearrange("p (h n) -> p h n", h=2)
        zs = tl(S, BF16)
        vaug = tl(NT * 768, BF16).rearrange("p (t n) -> p t n", t=NT)
        wv = tl(8 * 512, BF16).rearrange("p (c n) -> p c n", c=8)
        wf = tl(8 * 8, BF16).rearrange("p (c n) -> p c n", c=8)
        wp = [tl(8 * 384, BF16).rearrange("p (c n) -> p c n", c=8) for _ in range(2)]
        pt = [tl(512, BF16) for _ in range(6)]
        ext = tl(NT * 8 * 10, BF16).rearrange("p (t h e) -> p t h e", t=NT, h=8)
        gk = tl(128, F32)
        u1 = tl(128, F32)
        u2 = tl(128, F32)
        gp = tl(128, F32)
        srt = tl(128, F32)
        thr = tl(NT, F32)
        km = tl(16, F32)
        kmb = tl(16, BF16)
        rr = [tl(512, F32) for _ in range(2)]
        tm = [tl(512, F32) for _ in range(2)]
        b_q = [[Buf("q") for _ in range(NCH)] for _ in range(2)]
        b_k = [[Buf("k") for _ in range(NCH)] for _ in range(2)]
        b_z = [Buf("z") for _ in range(NCH)]
        b_v = [Buf("v") for _ in range(NT)]
        b_wv, b_wf = Buf("wv"), Buf("wf")
        b_wp = [Buf("wp0"), Buf("wp1")]
        b_pt = [Buf("pt") for _ in range(6)]
        b_ext = Buf("ext")
        b_sm = Buf("small")
        b_km = Buf("km")
        b_rr = [Buf("rr0"), Buf("rr1")]
        b_tm = [Buf("tm0"), Buf("tm1")]
        yt = yT[br]
        byt = b_yT[br]

        b_kc = Buf("kconst")
        P.op("gpsimd", lambda e: e.dma_start(out=kaug[64:74, :, :], in_=cd["kside"]), writes=[b_kc], dma_ch="kconst")
        P.op("gpsimd", lambda e: e.memset(vaug.rearrange("p t n -> p (t n)"), 1.0), writes=b_v)
        P.op("gpsimd", lambda e: e.memset(ext.rearrange("p t h e -> p (t h e)"), 0.0), writes=[b_ext])
        if br == 1:
            alq = tl(256, BF16)
            b_alq = Buf("alq")
            P.op("gpsimd", lambda e: e.dma_start(out=alq, in_=cd["alibi_q"]), writes=[b_alq], dma_ch="alq")
            P.op("gpsimd", lambda e: e.tensor_copy(out=ext[:, :, :, 8:10],
                                                   in_=alq.rearrange("p (t h e) -> p t h e", t=NT, h=8)),
                 reads=[b_alq], writes=[b_ext])
        wload(wv, b_wv, "wv", l, [(OFF[pre + "v"], 512, 0)])
        if br == 0:
            wload(wf, b_wf, "wf", l, [(OFF["a_f"], 8, 0)])

        def load_pair(j):
            wload(wp[j % 2], b_wp[j % 2], "wp%d" % (j % 2), l,
                  [(OFF[pre + "q"] + j * 128, 128, 0), (OFF[pre + "k"] + j * 128, 128, 128),
                   (OFF[pre + "z"] + j * 128, 128, 256)])
        load_pair(0)

        for t in range(NT):
            pi = t % 2
            for c in range(8):
                P.op("tensor", lambda e, c=c, t=t, pi=pi: e.matmul(
                    ps[pi][:, :], lhsT=hnT[:, c, t * 128:(t + 1) * 128], rhs=wv[:, c, :],
                    start=(c == 0), stop=(c == 7)), reads=[b_wv, b_hnT[t // 4]], writes=[bps[pi]])
            vv = vaug[:, t, :].rearrange("p (j n) -> p j n", j=4)
            pv = ps[pi].rearrange("p (j hh d) -> p j hh d", j=4, hh=2)
            P.op("scalar", lambda e, vv=vv, pv=pv: e.activation(out=vv[:, :, 0:64], in_=pv[:, :, 0, :], func=AF.Copy),
                 reads=[bps[pi]], writes=[b_v[t]])
            P.op("vector", lambda e, vv=vv, pv=pv: e.tensor_copy(out=vv[:, :, 128:192], in_=pv[:, :, 1, :]),
                 reads=[bps[pi]], writes=[b_v[t]])

        if br == 0:
            for t in range(NT):
                for c in range(8):
                    P.op("tensor", lambda e, c=c, t=t: e.matmul(
                        ps[6][:, t * 8:(t + 1) * 8], lhsT=hnT[:, c, t * 128:(t + 1) * 128], rhs=wf[:, c, :],
                        start=(c == 0), stop=(c == 7)), reads=[b_wf, b_hnT[t // 4]], writes=[bps[6]])
            P.op("vector", lambda e: e.tensor_tensor(out=u1, in0=ps[6][:, 0:128], in1=fbf[:, l, :], op=ALU.add),
                 reads=[bps[6]] + RC, writes=[b_sm])
            P.op("scalar", lambda e: e.activation(out=u2, in_=u1, func=AF.Exp, scale=-1.0), reads=[b_sm], writes=[b_sm])
            P.op("vector", lambda e: e.tensor_scalar_add(out=u2, in0=u2, scalar1=1.0), reads=[b_sm], writes=[b_sm])
            P.op("scalar", lambda e: e.activation(out=u1, in_=u2, func=AF.Ln), reads=[b_sm], writes=[b_sm])
            for t in range(NT):
                for tp in range(t + 1):
                    P.op("tensor", lambda e, t=t, tp=tp: e.matmul(
                        ps[5][:, t * 8:(t + 1) * 8], lhsT=(trif if tp == t else onesf),
                        rhs=u1[:, tp * 8:(tp + 1) * 8], start=(tp == 0), stop=(tp == t)),
                        reads=[b_sm] + RC, writes=[bps[5]])
            P.op("vector", lambda e: e.tensor_copy(out=gk, in_=ps[5][:, 0:128]), reads=[bps[5]], writes=[b_sm])
            gv = gk.rearrange("p (t h) -> p t h", t=NT)
            P.op("vector", lambda e: e.tensor_scalar(out=ext[:, :, :, 8], in0=gv, scalar1=-1.0, scalar2=None,
                                                     op0=ALU.mult), reads=[b_sm], writes=[b_ext])
            P.op("vector", lambda e: e.scalar_tensor_tensor(out=ext[:, :, :, 9], in0=gv, scalar=-1.0,
                                                            in1=ext[:, :, :, 8], op0=ALU.mult, op1=ALU.subtract),
                 reads=[b_sm, b_ext], writes=[b_ext])
            biast = gk
        else:
            biast = alibi_k

        def attention(hh, h):
            j = h // 2
            lo, hi_ = (0, 64) if hh == 0 else (64, 128)
            llo = 64 if hh == 0 else 0
            vc0 = j * 192 + (0 if hh == 0 else 64)
            tiles = []
            for qc in range(NCH):
                for kt in range(4 * qc + 4):
                    tiles.append((qc, kt))
            pend = []
            pidx = [0]

            def issue_pv(item):
                qc, kt, c0, n, pti = item
                po = 4 + (qc % 2)
                P.op("tensor", lambda e: e.matmul(
                    ps[po][:, c0:c0 + n], lhsT=vaug[:, kt, vc0:vc0 + 128], rhs=pt[pti][:, 0:n],
                    start=(kt == 0), stop=(kt == 4 * qc + 3)),
                    reads=[b_v[kt], b_pt[pti]], writes=[bps[po]])
                if kt == 4 * qc + 3:
                    fin(qc, po)

            def fin(qc, po):
                ri = qc % 2
                P.op("vector", lambda e: e.reciprocal(out=rr[ri][lo:hi_, :], in_=ps[po][llo:llo + 64, :]),
                     reads=[bps[po]], writes=[b_rr[ri]])
                P.op("vector", lambda e: e.tensor_tensor(out=tm[ri][lo:hi_, :], in0=ps[po][lo:hi_, :],
                                                         in1=rr[ri][lo:hi_, :], op=ALU.mult),
                     reads=[bps[po], b_rr[ri]], writes=[b_tm[ri]])
                P.op("gpsimd", lambda e: e.tensor_tensor(out=yt[lo:hi_, j, qc * 512:(qc + 1) * 512],
                                                         in0=tm[ri][lo:hi_, :], in1=zs[lo:hi_, qc * 512:(qc + 1) * 512],
                                                         op=ALU.mult),
                     reads=[b_tm[ri], b_z[qc]], writes=[byt[j][qc]])

            for (qc, kt) in tiles:
                d = kt - 4 * qc
                c0 = 0 if d < 0 else d * 128
                n = 512 - c0
                si = pidx[0] % 4
                pti = pidx[0] % 6
                pidx[0] += 1
                q0 = qc * 512 + c0
                P.op("tensor", lambda e, kt=kt, q0=q0, n=n, si=si: e.matmul(
                    ps[si][:, 0:n], lhsT=kaug[0:74, hh, kt * 128:(kt + 1) * 128], rhs=qaug[0:74, hh, q0:q0 + n],
                    start=True, stop=True),
                    reads=[b_k[hh][kt // 4], b_q[hh][qc], b_kc], writes=[bps[si]])
                P.op("scalar", lambda e, kt=kt, n=n, si=si, pti=pti: e.activation(
                    out=pt[pti][:, 0:n], in_=ps[si][:, 0:n], func=AF.Exp, bias=biast[:, kt * 8 + h:kt * 8 + h + 1]),
                    reads=[bps[si], b_sm] + RC, writes=[b_pt[pti]])
                if d >= 0:
                    P.op("gpsimd", lambda e, pti=pti: e.tensor_tensor(out=pt[pti][:, 0:128], in0=pt[pti][:, 0:128],
                                                                      in1=tri01, op=ALU.mult),
                         reads=[b_pt[pti]] + RC, writes=[b_pt[pti]])
                pend.append((qc, kt, c0, n, pti))
                if len(pend) > 3:
                    issue_pv(pend.pop(0))
            while pend:
                issue_pv(pend.pop(0))

        for j in range(4):
            w = wp[j % 2]
            bw = b_wp[j % 2]
            if j + 1 < 4:
                load_pair(j + 1)
            for chunk in range(NCH):
                cs = slice(chunk * 512, (chunk + 1) * 512)
                pi = chunk % 2
                proj_fm(w, bw, 0, chunk, pi)
                P.op("scalar", lambda e, cs=cs, pi=pi: e.activation(out=qaug[0:64, 0, cs], in_=ps[pi][0:64, :],
                                                                    func=AF.Copy, scale=0.125),
                     reads=[bps[pi]], writes=[b_q[0][chunk]])
                P.op("vector", lambda e, cs=cs, pi=pi: e.tensor_scalar(out=qaug[0:64, 1, cs], in0=ps[pi][64:128, :],
                                                                       scalar1=0.125, scalar2=None, op0=ALU.mult),
                     reads=[bps[pi]], writes=[b_q[1][chunk]])
            for chunk in range(NCH):
                cs = slice(chunk * 512, (chunk + 1) * 512)
                pi = chunk % 2
                proj_fm(w, bw, 128, chunk, pi)
                P.op("scalar", lambda e, cs=cs, pi=pi: e.activation(out=kaug[0:64, 0, cs], in_=ps[pi][0:64, :],
                                                                    func=AF.Copy),
                     reads=[bps[pi]], writes=[b_k[0][chunk]])
                P.op("vector", lambda e, cs=cs, pi=pi: e.tensor_copy(out=kaug[0:64, 1, cs], in_=ps[pi][64:128, :]),
                     reads=[bps[pi]], writes=[b_k[1][chunk]])
            for chunk in range(NCH):
                cs = slice(chunk * 512, (chunk + 1) * 512)
                pi = chunk % 2
                proj_fm(w, bw, 256, chunk, pi)
                P.op("scalar", lambda e, cs=cs, pi=pi: e.activation(out=zs[:, cs], in_=ps[pi][:, :], func=AF.Silu),
                     reads=[bps[pi]], writes=[b_z[chunk]])
            for hh in range(2):
                h = 2 * j + hh
                if br == 1:
                    P.op("vector", lambda e, hh=hh: e.tensor_reduce(
                        out=km[0:64, hh * 8:(hh + 1) * 8],
                        in_=kaug[0:64, hh, :].rearrange("p (b k) -> p b k", k=256), axis=AX.X, op=ALU.add),
                        reads=b_k[hh], writes=[b_km])
                    P.op("vector", lambda e, hh=hh: e.tensor_copy(out=kmb[0:64, hh * 8:(hh + 1) * 8],
                                                                  in_=km[0:64, hh * 8:(hh + 1) * 8]),
                         reads=[b_km], writes=[b_km])
                    for t in range(NT):
                        P.op("tensor", lambda e, t=t, hh=hh: e.matmul(
                            ps[6][:, t * 8:(t + 1) * 8], lhsT=qaug[0:64, hh, t * 128:(t + 1) * 128],
                            rhs=kmb[0:64, hh * 8:(hh + 1) * 8], start=True, stop=True),
                            reads=[b_q[hh][t // 4], b_km], writes=[bps[6]])
                    P.op("vector", lambda e: e.tensor_tensor(out=gp, in0=ps[6][:, 0:128], in1=mtable, op=ALU.add),
                         reads=[bps[6]] + RC, writes=[b_sm])
                    for t in range(NT):
                        P.op("vector", lambda e, t=t: e.max(out=srt[:, t * 8:(t + 1) * 8], in_=gp[:, t * 8:(t + 1) * 8]),
                             reads=[b_sm], writes=[b_sm])
                    P.op("vector", lambda e: e.tensor_scalar(
                        out=thr, in0=srt.rearrange("p (t k) -> p t k", k=8)[:, :, 3], scalar1=-1e29, scalar2=None,
                        op0=ALU.max), reads=[b_sm], writes=[b_sm])
                    for t in range(NT):
                        P.op("vector", lambda e, t=t, h=h: e.tensor_scalar(
                            out=ext[:, t, h, 0:8], in0=gp[:, t * 8:(t + 1) * 8], scalar1=thr[:, t:t + 1],
                            scalar2=-BIG, op0=ALU.is_lt, op1=ALU.mult), reads=[b_sm], writes=[b_ext])
                for chunk in range(NCH):
                    pb = chunk % 2
                    for tt in range(4):
                        t = chunk * 4 + tt
                        P.op("tensor", lambda e, t=t, tt=tt, pb=pb, h=h: e.matmul(
                            psb[pb][0:10, tt * 128:(tt + 1) * 128], lhsT=ext[:, t, h, :], rhs=ident,
                            start=True, stop=True),
                            reads=[b_ext] + RC, writes=[bpsb[pb]])
                    P.op("vector", lambda e, chunk=chunk, pb=pb, hh=hh: e.tensor_copy(
                        out=qaug[64:74, hh, chunk * 512:(chunk + 1) * 512], in_=psb[pb][0:10, :]),
                        reads=[bpsb[pb]], writes=[b_q[hh][chunk]])
                attention(hh, h)
        P.barrier()

    def mlstm_stage(s, l):
        cur[0] = stage0
        qkT = tl(4 * S, BF16).rearrange("p (c n) -> p c n", c=4)
        cst = tl(S + 4, F32)
        acc = tl(S, F32)
        vC = tl(NT * 512, BF16).rearrange("p (t n) -> p t n", t=NT)
        oT = tl(S, BF16)
        zT = tl(S, BF16)
        faug = tl(2 * S, BF16).rearrange("p (h n) -> p h n", h=2)
        wv = tl(8 * 512, BF16).rearrange("p (c n) -> p c n", c=8)
        wf = tl(8 * 8, BF16).rearrange("p (c n) -> p c n", c=8)
        wq = [tl(8 * 128, BF16).rearrange("p (c n) -> p c n", c=8) for _ in range(2)]
        wp = [tl(8 * 256, BF16).rearrange("p (c n) -> p c n", c=8) for _ in range(2)]
        dt_ = [tl(512, F32) for _ in range(3)]
        st_ = [tl(512, BF16) for _ in range(6)]
        ext = tl(NT * 4 * 4, BF16).rearrange("p (t h e) -> p t h e", t=NT, h=4)
        u1 = tl(128, F32)
        spf = tl(64, F32)
        e1 = tl(64, F32)
        gf = tl(64, F32)
        bias_c = tl(64, F32)
        r1 = tl(64, F32)
        fa = [tl(512, F32) for _ in range(2)]
        fb = [tl(512, F32) for _ in range(2)]
        fsq = [tl(512, BF16) for _ in range(2)]
        b_qk = [[Buf("qk") for _ in range(NCH)] for _ in range(4)]
        b_cst, b_acc = Buf("cst"), Buf("acc")
        b_v = [Buf("v") for _ in range(NT)]
        b_o = [Buf("o") for _ in range(NCH)]
        b_z = [Buf("z") for _ in range(NCH)]
        b_fa = [[Buf("faug") for _ in range(NCH)] for _ in range(2)]
        b_wv, b_wf = Buf("wv"), Buf("wf")
        b_wq = [Buf("wq0"), Buf("wq1")]
        b_wp = [Buf("wp0"), Buf("wp1")]
        b_dt = [Buf("dt") for _ in range(3)]
        b_st = [Buf("st") for _ in range(6)]
        b_ext, b_sm = Buf("ext"), Buf("small")
        b_f = [[Buf("fa"), Buf("fb"), Buf("fsq")] for _ in range(2)]
        yt = yT[2]
        byt = b_yT[2]

        P.op("gpsimd", lambda e: e.memset(cst[:, 0:4], 0.0), writes=[b_cst])
        P.op("gpsimd", lambda e: e.memset(faug.rearrange("p h n -> p (h n)"), 0.0), writes=[x for y in b_fa for x in y])
        wload(wv, b_wv, "wv", l, [(OFF["c_v"], 512, 0)])
        wload(wf, b_wf, "wf", l, [(OFF["c_if"], 8, 0)])
        wload(wq[0], b_wq[0], "wq0", l, [(OFF["c_qk"], 128, 0)])

        for t in range(NT):
            pi = t % 2
            for c in range(8):
                P.op("tensor", lambda e, c=c, t=t, pi=pi: e.matmul(
                    ps[pi][:, :], lhsT=hnT[:, c, t * 128:(t + 1) * 128], rhs=wv[:, c, :],
                    start=(c == 0), stop=(c == 7)), reads=[b_wv, b_hnT[t // 4]], writes=[bps[pi]])
            eng = "scalar" if t % 2 == 0 else "vector"
            if eng == "scalar":
                P.op("scalar", lambda e, t=t, pi=pi: e.activation(out=vC[:, t, :], in_=ps[pi][:, :], func=AF.Copy),
                     reads=[bps[pi]], writes=[b_v[t]])
            else:
                P.op("vector", lambda e, t=t, pi=pi: e.tensor_copy(out=vC[:, t, :], in_=ps[pi][:, :]),
                     reads=[bps[pi]], writes=[b_v[t]])

        for t in range(NT):
            for c in range(8):
                P.op("tensor", lambda e, c=c, t=t: e.matmul(
                    ps[6][:, t * 8:(t + 1) * 8], lhsT=hnT[:, c, t * 128:(t + 1) * 128], rhs=wf[:, c, :],
                    start=(c == 0), stop=(c == 7)), reads=[b_wf, b_hnT[t // 4]], writes=[bps[6]])
        P.op("vector", lambda e: e.tensor_tensor(out=u1, in0=ps[6][:, 0:128], in1=bif[:, l, :], op=ALU.add),
             reads=[bps[6]] + RC, writes=[b_sm])
        u1v = u1.rearrange("p (t g) -> p t g", t=NT)
        e1v = e1.rearrange("p (t g) -> p t g", t=NT)
        P.op("scalar", lambda e: e.activation(out=e1v, in_=u1v[:, :, 4:8], func=AF.Exp, scale=-1.0),
             reads=[b_sm], writes=[b_sm])
        P.op("vector", lambda e: e.tensor_scalar_add(out=e1, in0=e1, scalar1=1.0), reads=[b_sm], writes=[b_sm])
        P.op("scalar", lambda e: e.activation(out=spf, in_=e1, func=AF.Ln), reads=[b_sm], writes=[b_sm])
        for t in range(NT):
            for tp in range(t + 1):
                P.op("tensor", lambda e, t=t, tp=tp: e.matmul(
                    ps[5][:, t * 4:(t + 1) * 4], lhsT=(trif if tp == t else onesf),
                    rhs=spf[:, tp * 4:(tp + 1) * 4], start=(tp == 0), stop=(tp == t)),
                    reads=[b_sm] + RC, writes=[bps[5]])
        P.op("vector", lambda e: e.tensor_copy(out=gf, in_=ps[5][:, 0:64]), reads=[bps[5]], writes=[b_sm])
        gfv = gf.rearrange("p (t h) -> p t h", t=NT)
        P.op("vector", lambda e: e.tensor_tensor(out=bias_c.rearrange("p (t h) -> p t h", t=NT), in0=u1v[:, :, 0:4],
                                                 in1=gfv, op=ALU.add), reads=[b_sm], writes=[b_sm])
        r1v = r1.rearrange("p (t h) -> p t h", t=NT)
        P.op("vector", lambda e: e.tensor_scalar(out=ext[:, :, :, 0], in0=gfv, scalar1=-1.0, scalar2=None, op0=ALU.mult),
             reads=[b_sm], writes=[b_ext])
        P.op("vector", lambda e: e.scalar_tensor_tensor(out=r1v, in0=gfv, scalar=-1.0, in1=ext[:, :, :, 0],
                                                        op0=ALU.mult, op1=ALU.subtract),
             reads=[b_sm, b_ext], writes=[b_sm])
        P.op("vector", lambda e: e.tensor_copy(out=ext[:, :, :, 1], in_=r1v), reads=[b_sm, b_ext], writes=[b_ext])
        P.op("vector", lambda e: e.tensor_tensor(out=ext[:, :, :, 2], in0=r1v, in1=ext[:, :, :, 1], op=ALU.subtract),
             reads=[b_sm, b_ext], writes=[b_ext])
        P.op("vector", lambda e: e.memset(ext[:, :, :, 3], 0.0), reads=[b_ext], writes=[b_ext])
        for h in range(4):
            for chunk in range(NCH):
                pb = chunk % 2
                for tt in range(4):
                    t = chunk * 4 + tt
                    P.op("tensor", lambda e, t=t, tt=tt, pb=pb, h=h: e.matmul(
                        psb[pb][0:4, tt * 128:(tt + 1) * 128], lhsT=ext[:, t, h, :], rhs=ident,
                        start=True, stop=True),
                        reads=[b_ext] + RC, writes=[bpsb[pb]])
                r0 = 32 * (h % 2)
                P.op("vector", lambda e, chunk=chunk, pb=pb, h=h, r0=r0: e.tensor_copy(
                    out=faug[r0:r0 + 4, h // 2, chunk * 512:(chunk + 1) * 512], in_=psb[pb][0:4, :]),
                    reads=[bpsb[pb]], writes=[b_fa[h // 2][chunk]])

        for cc in range(4):
            w = wq[cc % 2]
            bw = b_wq[cc % 2]
            if cc + 1 < 4:
                wload(wq[(cc + 1) % 2], b_wq[(cc + 1) % 2], "wq%d" % ((cc + 1) % 2), l,
                      [(OFF["c_qk"] + (cc + 1) * 128, 128, 0)])
            for chunk in range(NCH):
                pi = chunk % 2
                proj_fm(w, bw, 0, chunk, pi)
                eng = "scalar" if chunk % 2 == 0 else "vector"
                dst = cst[:, 4 + chunk * 512:4 + (chunk + 1) * 512]
                if eng == "scalar":
                    P.op("scalar", lambda e, dst=dst, pi=pi: e.activation(out=dst, in_=ps[pi][:, :], func=AF.Copy),
                         reads=[bps[pi]], writes=[b_cst])
                else:
                    P.op("vector", lambda e, dst=dst, pi=pi: e.tensor_copy(out=dst, in_=ps[pi][:, :]),
                         reads=[bps[pi]], writes=[b_cst])
            P.op("gpsimd", lambda e, cc=cc: e.tensor_scalar(out=acc, in0=cst[:, 4:4 + S], scalar1=cw[:, l, cc, 3:4],
                                                            scalar2=None, op0=ALU.mult),
                 reads=[b_cst] + RC, writes=[b_acc])
            for jj in (2, 1, 0):
                sh = 3 - jj
                P.op("vector", lambda e, cc=cc, jj=jj, sh=sh: e.scalar_tensor_tensor(
                    out=acc, in0=cst[:, 4 - sh:4 - sh + S], scalar=cw[:, l, cc, jj:jj + 1], in1=acc,
                    op0=ALU.mult, op1=ALU.add), reads=[b_cst, b_acc] + RC, writes=[b_acc])
            for chunk in range(NCH):
                P.op("scalar", lambda e, cc=cc, chunk=chunk: e.activation(
                    out=qkT[:, cc, chunk * 512:(chunk + 1) * 512], in_=acc[:, chunk * 512:(chunk + 1) * 512],
                    func=AF.Silu), reads=[b_acc], writes=[b_qk[cc][chunk]])

        def load_head(h):
            wload(wp[h % 2], b_wp[h % 2], "wp%d" % (h % 2), l,
                  [(OFF["c_o"] + h * 128, 128, 0), (OFF["c_z"] + h * 128, 128, 128)])
        load_head(0)

        def attention(h):
            pb0 = 64 * (h % 2)
            qch = h // 2
            kch = 2 + h // 2
            r0 = 32 * (h % 2)
            fs = h // 2
            tiles = [(qc, kt) for qc in range(NCH) for kt in range(4 * qc + 4)]
            pend = []
            cnt = [0]

            def issue_pv(item):
                qc, kt, c0, n, sti = item
                P.op("tensor", lambda e: e.matmul(ps[4][:, c0:c0 + n], lhsT=vC[:, kt, h * 128:(h + 1) * 128],
                                                  rhs=st_[sti][:, 0:n], start=(kt == 0), stop=(kt == 4 * qc + 3)),
                     reads=[b_v[kt], b_st[sti]], writes=[bps[4]])
                P.op("tensor", lambda e: e.matmul(ps[5][:, c0:c0 + n], lhsT=onesb, rhs=st_[sti][:, 0:n],
                                                  start=(kt == 0), stop=(kt == 4 * qc + 3)),
                     reads=[b_st[sti]] + RC, writes=[bps[5]])
                if kt == 4 * qc + 3:
                    fin(qc)

            def fin(qc):
                fi = qc % 2
                cs = slice(qc * 512, (qc + 1) * 512)
                bf_a, bf_b, bf_s = b_f[fi]
                P.op("scalar", lambda e: e.activation(out=fa[fi], in_=ps[5][:, :], func=AF.Abs),
                     reads=[bps[5]], writes=[bf_a])
                P.op("vector", lambda e: e.tensor_scalar_max(out=fa[fi], in0=fa[fi], scalar1=1.0),
                     reads=[bf_a], writes=[bf_a])
                P.op("vector", lambda e: e.reciprocal(out=fa[fi], in_=fa[fi]), reads=[bf_a], writes=[bf_a])
                P.op("vector", lambda e: e.tensor_tensor(out=fb[fi], in0=ps[4][:, :], in1=fa[fi], op=ALU.mult),
                     reads=[bps[4], bf_a], writes=[bf_b])
                P.op("gpsimd", lambda e: e.tensor_tensor(out=fb[fi], in0=fb[fi], in1=oT[:, cs], op=ALU.mult),
                     reads=[bf_b, b_o[qc]], writes=[bf_b])
                P.op("scalar", lambda e: e.activation(out=fsq[fi], in_=fb[fi], func=AF.Square),
                     reads=[bf_b], writes=[bf_s])
                P.op("tensor", lambda e: e.matmul(ps[6][:, :], lhsT=o128b, rhs=fsq[fi], start=True, stop=True),
                     reads=[bf_s] + RC, writes=[bps[6]])
                P.op("scalar", lambda e: e.activation(out=fa[fi], in_=ps[6][:, :], func=AF.Ln, bias=epsc),
                     reads=[bps[6]] + RC, writes=[bf_a])
                P.op("scalar", lambda e: e.activation(out=fa[fi], in_=fa[fi], func=AF.Exp, scale=-0.5),
                     reads=[bf_a], writes=[bf_a])
                P.op("vector", lambda e: e.tensor_tensor(out=fb[fi], in0=fb[fi], in1=fa[fi], op=ALU.mult),
                     reads=[bf_a, bf_b], writes=[bf_b])
                P.op("vector", lambda e: e.scalar_tensor_tensor(out=yt[:, h, cs], in0=fb[fi], scalar=hg[:, l, h:h + 1],
                                                                in1=zT[:, cs], op0=ALU.mult, op1=ALU.mult),
                     reads=[bf_b, b_z[qc]] + RC, writes=[byt[h][qc]])

            for (qc, kt) in tiles:
                d = kt - 4 * qc
                c0 = 0 if d < 0 else d * 128
                n = 512 - c0
                k_ = cnt[0]
                cnt[0] += 1
                si = (2, 3, 6)[k_ % 3]
                ei = (0, 1, 7)[k_ % 3]
                di = k_ % 3
                sti = k_ % 6
                q0 = qc * 512 + c0
                P.op("tensor", lambda e, kt=kt, q0=q0, n=n, si=si: e.matmul(
                    ps[si][:, 0:n], lhsT=qkT[pb0:pb0 + 64, kch, kt * 128:(kt + 1) * 128],
                    rhs=qkT[pb0:pb0 + 64, qch, q0:q0 + n], start=True, stop=True),
                    reads=[b_qk[kch][kt // 4], b_qk[qch][qc]], writes=[bps[si]])
                P.op("tensor", lambda e, q0=q0, n=n, ei=ei: e.matmul(
                    ps[ei][:, 0:n], lhsT=onesb[r0:r0 + 4, :], rhs=faug[r0:r0 + 4, fs, q0:q0 + n],
                    start=True, stop=True), reads=[b_fa[fs][qc]] + RC, writes=[bps[ei]])
                P.op("scalar", lambda e, kt=kt, n=n, ei=ei, di=di: e.activation(
                    out=dt_[di][:, 0:n], in_=ps[ei][:, 0:n], func=AF.Exp, bias=bias_c[:, kt * 4 + h:kt * 4 + h + 1]),
                    reads=[bps[ei], b_sm], writes=[b_dt[di]])
                P.op("vector", lambda e, n=n, si=si, di=di, sti=sti: e.scalar_tensor_tensor(
                    out=st_[sti][:, 0:n], in0=ps[si][:, 0:n], scalar=0.125, in1=dt_[di][:, 0:n],
                    op0=ALU.mult, op1=ALU.mult), reads=[bps[si], b_dt[di]], writes=[b_st[sti]])
                if d >= 0:
                    P.op("gpsimd", lambda e, sti=sti: e.tensor_tensor(out=st_[sti][:, 0:128], in0=st_[sti][:, 0:128],
                                                                      in1=tri01, op=ALU.mult),
                         reads=[b_st[sti]] + RC, writes=[b_st[sti]])
                pend.append((qc, kt, c0, n, sti))
                if len(pend) > 3:
                    issue_pv(pend.pop(0))
            while pend:
                issue_pv(pend.pop(0))

        for h in range(4):
            w = wp[h % 2]
            bw = b_wp[h % 2]
            if h + 1 < 4:
                load_head(h + 1)
            for chunk in range(NCH):
                cs = slice(chunk * 512, (chunk + 1) * 512)
                pi = chunk % 2
                proj_fm(w, bw, 0, chunk, pi)
                P.op("scalar", lambda e, cs=cs, pi=pi: e.activation(out=oT[:, cs], in_=ps[pi][:, :], func=AF.Sigmoid),
                     reads=[bps[pi]], writes=[b_o[chunk]])
            for chunk in range(NCH):
                cs = slice(chunk * 512, (chunk + 1) * 512)
                pi = chunk % 2
                proj_fm(w, bw, 128, chunk, pi)
                P.op("scalar", lambda e, cs=cs, pi=pi: e.activation(out=zT[:, cs], in_=ps[pi][:, :], func=AF.Silu),
                     reads=[bps[pi]], writes=[b_z[chunk]])
            attention(h)
        P.barrier()

    def merge_stage(s, l, finals):
        cur[0] = stage0
        wo = tl(8 * D, BF16).rearrange("p (c n) -> p c n", c=8)
        wb = tl(12 * D, BF16).rearrange("p (c n) -> p c n", c=12)
        wg = [tl(8 * 512, BF16).rearrange("p (c n) -> p c n", c=8) for _ in range(2)]
        mbf = tl(8 * 1024, BF16).rearrange("p (c n) -> p c n", c=8)
        macc = [[tl(512, F32) for _ in range(2)] for _ in range(4)]
        gs = [tl(512, F32) for _ in range(2)]
        tmp = [tl(512, F32) for _ in range(2)]
        xr = [tl(D, F32) for _ in range(2)]
        jk = tl(D, BF16)
        ss = tl(NT, F32)
        b_wo, b_wb = Buf("wo"), Buf("wb")
        b_wg = [Buf("wg0"), Buf("wg1")]
        b_mbf = [Buf("mbf") for _ in range(8)]
        b_macc = [[Buf("macc") for _ in range(2)] for _ in range(4)]
        b_gs = [Buf("gs0"), Buf("gs1")]
        b_tmp = [Buf("tmp0"), Buf("tmp1")]
        b_xr = [Buf("xr0"), Buf("xr1")]
        b_jk, b_ss = Buf("jk"), Buf("ss")
        src = x_in[s] if l == 0 else xs_d[s]

        P.op("gpsimd", lambda e: [e.dma_start(out=wb[:, n * 4:(n + 1) * 4, :],
                                              in_=w_br[l, n].rearrange("(c p) d -> p c d", p=128)) for n in range(3)],
             writes=[b_wb], dma_ch="wb", dma_n=3)
        gcnt = [0]
        it = [0]
        first = True
        for half in range(2):
            for ftg in range(2):
                for n in range(3):
                    gi = gcnt[0] % 2
                    gcnt[0] += 1
                    wload(wg[gi], b_wg[gi], "wg%d" % gi, l, [(OFF["gates"] + n * 1024 + ftg * 512, 512, 0)])
                    if first:
                        first = False
                        P.op("gpsimd", lambda e: e.dma_start(out=wo, in_=w_out[l].rearrange("(c p) n -> p c n", p=128)),
                             writes=[b_wo], dma_ch="wo")
                    for fl in range(4):
                        ft = ftg * 4 + fl
                        for cc in range(2):
                            chunk = half * 2 + cc
                            cs = slice(chunk * 512, (chunk + 1) * 512)
                            k_ = it[0] % 2
                            it[0] += 1
                            pg = k_
                            pbk = 2 + k_
                            for c in range(8):
                                P.op("tensor", lambda e, c=c, gi=gi, cs=cs, pg=pg, fl=fl: e.matmul(
                                    ps[pg][:, :], lhsT=wg[gi][:, c, fl * 128:(fl + 1) * 128], rhs=hnT[:, c, cs],
                                    start=(c == 0), stop=(c == 7)),
                                    reads=[b_wg[gi], b_hnT[chunk]], writes=[bps[pg]])
                            for wc in range(4):
                                P.op("tensor", lambda e, wc=wc, n=n, ft=ft, cs=cs, pbk=pbk: e.matmul(
                                    ps[pbk][:, :], lhsT=wb[:, n * 4 + wc, ft * 128:(ft + 1) * 128], rhs=yT[n][:, wc, cs],
                                    start=(wc == 0), stop=(wc == 3)),
                                    reads=[b_wb, b_yT[n][wc][chunk]], writes=[bps[pbk]])
                            P.op("scalar", lambda e, k_=k_, pg=pg: e.activation(out=gs[k_], in_=ps[pg][:, :], func=AF.Sigmoid),
                                 reads=[bps[pg]], writes=[b_gs[k_]])
                            ma, bma = macc[fl][cc], b_macc[fl][cc]
                            if n == 0:
                                P.op("vector", lambda e, k_=k_, pbk=pbk, ma=ma: e.tensor_tensor(
                                    out=ma, in0=ps[pbk][:, :], in1=gs[k_], op=ALU.mult),
                                    reads=[bps[pbk], b_gs[k_]], writes=[bma])
                            else:
                                P.op("vector", lambda e, k_=k_, pbk=pbk: e.tensor_tensor(
                                    out=tmp[k_], in0=ps[pbk][:, :], in1=gs[k_], op=ALU.mult),
                                    reads=[bps[pbk], b_gs[k_]], writes=[b_tmp[k_]])
                                if n == 1:
                                    P.op("gpsimd", lambda e, k_=k_, ma=ma: e.tensor_tensor(out=ma, in0=ma, in1=tmp[k_], op=ALU.add),
                                         reads=[b_tmp[k_], bma], writes=[bma])
                                else:
                                    P.op("gpsimd", lambda e, k_=k_, ma=ma, ft=ft, cc=cc: e.tensor_tensor(
                                        out=mbf[:, ft, cc * 512:(cc + 1) * 512], in0=ma, in1=tmp[k_], op=ALU.add),
                                        reads=[b_tmp[k_], bma], writes=[b_mbf[ft]])
            for tt in range(8):
                t = half * 8 + tt
                i = tt % 2
                P.op("sync", lambda e, i=i, t=t: e.dma_start(out=xr[i], in_=src[t * 128:(t + 1) * 128, :]),
                     writes=[b_xr[i]], dma_ch="xr%d" % i)
                for hc in range(2):
                    po = 4 + hc
                    for fc in range(8):
                        P.op("tensor", lambda e, fc=fc, tt=tt, hc=hc, po=po: e.matmul(
                            ps[po][:, :], lhsT=mbf[:, fc, tt * 128:(tt + 1) * 128], rhs=wo[:, fc, hc * 512:(hc + 1) * 512],
                            start=(fc == 0), stop=(fc == 7)), reads=[b_wo, b_mbf[fc]], writes=[bps[po]])
                    P.op("vector", lambda e, i=i, hc=hc, po=po: e.tensor_tensor(
                        out=xr[i][:, hc * 512:(hc + 1) * 512], in0=ps[po][:, :], in1=xr[i][:, hc * 512:(hc + 1) * 512],
                        op=ALU.add), reads=[bps[po], b_xr[i]], writes=[b_xr[i]])
                if l == 0:
                    P.op("sync", lambda e, i=i, t=t: e.dma_start(out=xs_d[s, t * 128:(t + 1) * 128, :], in_=xr[i]),
                         reads=[b_xr[i]], dma_ch="st%d" % i)
                    if dbg and s == 0:
                        finals.append(P.op("sync", lambda e, i=i, t=t: e.dma_start(
                            out=dbg_d["dbg_x1"][t * 128:(t + 1) * 128, :], in_=xr[i]), reads=[b_xr[i]], dma_ch="dbgx"))
                else:
                    P.op("scalar", lambda e, i=i, t=t: e.activation(out=jk, in_=xr[i], func=AF.Square,
                                                                    accum_out=ss[:, t:t + 1]),
                         reads=[b_xr[i]], writes=[b_jk, b_ss])
                    P.op("scalar", lambda e, t=t: e.activation(out=ss[:, t:t + 1], in_=ss[:, t:t + 1], func=AF.Ln,
                                                               bias=epsc, scale=1.0 / D),
                         reads=[b_ss] + RC, writes=[b_ss])
                    P.op("scalar", lambda e, t=t: e.activation(out=ss[:, t:t + 1], in_=ss[:, t:t + 1], func=AF.Exp,
                                                               scale=-0.5),
                         reads=[b_ss], writes=[b_ss])
                    P.op("vector", lambda e, i=i, t=t: e.scalar_tensor_tensor(
                        out=xr[i], in0=xr[i], scalar=ss[:, t:t + 1], in1=fgrep, op0=ALU.mult, op1=ALU.mult),
                        reads=[b_xr[i], b_ss] + RC, writes=[b_xr[i]])
                    finals.append(P.op("sync", lambda e, i=i, t=t: e.dma_start(
                        out=out_d[s, t * 128:(t + 1) * 128, :], in_=xr[i]), reads=[b_xr[i]], dma_ch="st%d" % i))
        P.barrier()

    def dump(nm, src_ap, bufs, finals):
        finals.append(P.op("gpsimd", lambda e: e.dma_start(out=dbg_d[nm], in_=src_ap), reads=bufs, dma_ch="dbg_" + nm))

    finals = []
    import os
    limit = int(os.environ.get("MK_LIMIT", "1000"))
    nst = [0]

    def go(fn, *a):
        if nst[0] < limit:
            fn(*a)
        nst[0] += 1

    for s in range(NSEQ):
        for l in range(DEPTH):
            go(norm_stage, s, l)
            if dbg and s == 0 and l == 0:
                dump("dbg_hnT", hnT.rearrange("p c n -> p (c n)"), b_hnT, finals)
                P.barrier()
            go(attn_stage, s, l, 0)
            go(attn_stage, s, l, 1)
            go(mlstm_stage, s, l)
            if dbg and s == 0 and l == 0:
                for bi, nm in enumerate(("dbg_ya", "dbg_yb", "dbg_yc")):
                    dump(nm, yT[bi].rearrange("p c n -> p (c n)"), [x for y in b_yT[bi] for x in y], finals)
                P.barrier()
            go(merge_stage, s, l, finals)
            P.barrier(new_epoch=True)
    P.emit(final_waits=finals)
    return nc


_CACHE = {}


def prep_inputs(inputs):
    f = np.float32
    norm_g = np.asarray(inputs["norm_g"], f)
    gfull = np.zeros((128, DEPTH, 8, 128), f)
    for l in range(DEPTH):
        gfull[:, l, :, :] = norm_g[l].reshape(8, 128).T[:, :, None]
    shared = {
        "w_in": np.ascontiguousarray(inputs["w_in"], f),
        "w_branch": np.ascontiguousarray(inputs["w_branch"], f),
        "w_out": np.ascontiguousarray(inputs["w_out"], f),
        "gfull": gfull.reshape(128, -1),
        "fgrep": np.ascontiguousarray(np.broadcast_to(np.asarray(inputs["final_norm_g"], f)[None, :], (128, D))),
    }
    fbf = np.zeros((128, DEPTH, NT, 8), f)
    bif = np.zeros((128, DEPTH, NT, 8), f)
    for l in range(DEPTH):
        fbf[:, l, :, :] = np.asarray(inputs["fox_b_f"], f)[l][None, None, :]
        bif[:, l, :, 0:4] = np.asarray(inputs["mlstm_b_i"], f)[l][None, None, :]
        bif[:, l, :, 4:8] = np.asarray(inputs["mlstm_b_f"], f)[l][None, None, :]
    shared["fbf_rep"] = fbf.reshape(128, -1)
    shared["bif_rep"] = bif.reshape(128, -1)
    cwv = np.asarray(inputs["mlstm_conv_w"], f)
    cw = np.zeros((128, DEPTH, 4, 4), f)
    for l in range(DEPTH):
        cw[:, l, :, :] = cwv[l].reshape(4, 4, 128).transpose(2, 1, 0)
    shared["cw"] = cw.reshape(128, -1)
    hgv = np.asarray(inputs["mlstm_head_g"], f)
    hg = np.zeros((128, DEPTH, 4), f)
    for l in range(DEPTH):
        hg[:, l, :] = hgv[l].reshape(4, 128).T
    shared["hg"] = hg.reshape(128, -1)
    for k, v in host_consts().items():
        shared["c_" + k] = np.ascontiguousarray(v, f)
    return shared


def kernel(**inputs):
    x = np.ascontiguousarray(inputs["x"], np.float32)
    shared = prep_inputs(inputs)
    if "nc" not in _CACHE:
        _CACHE["nc"] = build(False)
    nc = _CACHE["nc"]
    in_maps = []
    for c in range(8):
        m = dict(shared)
        m["x"] = np.ascontiguousarray(x[2 * c:2 * c + 2])
        in_maps.append(m)
    res = run_bass_kernel_spmd(nc, in_maps, core_ids=list(range(8)))
    out = np.concatenate([np.asarray(r["out"], np.float32) for r in res.results], axis=0)
    return out
```

```python
import contextlib
import numpy as np
import concourse.bass as bass
import concourse.mybir as mybir
from concourse.bass_utils import run_bass_kernel_spmd

F32 = mybir.dt.float32
BF16 = mybir.dt.bfloat16
U8 = mybir.dt.uint8
AF = mybir.ActivationFunctionType
ALU = mybir.AluOpType
AX = mybir.AxisListType

S = 2048
D = 1024
NT = 16
NCH = 4
DEPTH = 2
NSEQ = 2
INW = 9232
OFF = dict(a_q=0, a_k=512, a_v=1024, a_z=1536, a_f=2048, b_q=2056, b_k=2568, b_v=3080,
           b_z=3592, c_qk=4104, c_v=4616, c_o=5128, c_z=5640, c_if=6152, gates=6160)
EPS = 1e-6
BIG = 30000.0


class Buf:
    __slots__ = ("name", "last_w", "reads")

    def __init__(self, name):
        self.name = name
        self.last_w = None
        self.reads = []


class Op:
    __slots__ = ("eng", "fn", "deps", "signal", "sigval", "dma_ch", "dma_n", "epoch", "idx")


class Prog:
    ENGS = ("tensor", "vector", "scalar", "gpsimd", "sync")

    def __init__(self, nc):
        self.nc = nc
        self.q = {e: [] for e in self.ENGS}
        self.ops = []
        self.epoch = 0
        self.last_real = {e: None for e in self.ENGS}
        self.last_dma = {}
        self.channels = []

    def op(self, eng, fn, reads=(), writes=(), dma_ch=None, dma_n=1, extra=()):
        o = Op()
        o.eng = eng
        o.fn = fn
        o.signal = False
        o.sigval = None
        o.dma_ch = dma_ch
        o.dma_n = dma_n
        o.epoch = self.epoch
        o.idx = len(self.ops)
        deps = set(extra)
        for b in reads:
            if b.last_w is not None:
                deps.add(b.last_w)
        for b in writes:
            if b.last_w is not None:
                deps.add(b.last_w)
            deps.update(b.reads)
        deps.discard(o)
        best = {}
        for d in deps:
            if d.fn is None:
                continue
            if d.dma_ch is not None:
                key = ("dma", d.dma_ch)
            else:
                if d.eng == "tensor" and eng == "tensor" and dma_ch is None:
                    continue
                key = ("eng", d.eng, d.epoch)
            if key not in best or best[key].idx < d.idx:
                best[key] = d
        o.deps = list(best.values())
        for b in reads:
            b.reads.append(o)
        for b in writes:
            b.last_w = o
            b.reads = []
        self.q[eng].append(o)
        self.ops.append(o)
        if fn is not None:
            if dma_ch is not None:
                self.last_dma[dma_ch] = o
                if dma_ch not in self.channels:
                    self.channels.append(dma_ch)
            else:
                self.last_real[eng] = o
        return o

    def barrier(self, new_epoch=False):
        tgt = [o for o in self.last_real.values() if o is not None] + list(self.last_dma.values())
        for e in self.ENGS:
            self.op(e, None, extra=tgt)
        if new_epoch:
            self.epoch += 1

    def emit(self, final_waits=()):
        nc = self.nc
        for o in self.ops:
            for d in o.deps:
                d.signal = True
        for o in final_waits:
            o.signal = True
        with contextlib.ExitStack() as st:
            esem = {}
            for ep in range(self.epoch + 1):
                for e in self.ENGS:
                    esem[(ep, e)] = st.enter_context(nc.semaphore("s%d_%s" % (ep, e)))
            dsem = {c: st.enter_context(nc.semaphore("d_" + c)) for c in self.channels}
            cnt = {}
            chcnt = {}
            for e in self.ENGS:
                for o in self.q[e]:
                    if o.fn is None:
                        continue
                    if o.dma_ch is not None:
                        chcnt[o.dma_ch] = chcnt.get(o.dma_ch, 0) + 16 * o.dma_n
                        o.sigval = (dsem[o.dma_ch], chcnt[o.dma_ch])
                    elif o.signal:
                        k = (o.epoch, e)
                        cnt[k] = cnt.get(k, 0) + 1
                        o.sigval = (esem[k], cnt[k])
            prog = self

            def run(e, engine):
                seen = {}
                for o in prog.q[e]:
                    need = {}
                    for d in o.deps:
                        if d.sigval is None:
                            continue
                        s, v = d.sigval
                        k = id(s)
                        if seen.get(k, 0) >= v:
                            continue
                        if k not in need or need[k][1] < v:
                            need[k] = (s, v)
                    for k, (s, v) in need.items():
                        engine.wait_ge(s, v)
                        seen[k] = v
                    if o.fn is None:
                        continue
                    ins = o.fn(engine)
                    if o.dma_ch is not None:
                        lst = ins if isinstance(ins, (list, tuple)) else [ins]
                        assert len(lst) == o.dma_n, (len(lst), o.dma_n)
                        for i_ in lst:
                            i_.then_inc(o.sigval[0], 16)
                    elif o.signal:
                        ins.then_inc(o.sigval[0], 1)
                if e == "sync":
                    for o in final_waits:
                        s, v = o.sigval
                        engine.wait_ge(s, v)

            with nc.Block() as block:
                @block.tensor
                def _(eng):
                    run("tensor", eng)

                @block.vector
                def _(eng):
                    run("vector", eng)

                @block.scalar
                def _(eng):
                    run("scalar", eng)

                @block.gpsimd
                def _(eng):
                    run("gpsimd", eng)

                @block.sync
                def _(eng):
                    run("sync", eng)


def host_consts():
    c = {}
    c["ident"] = np.eye(128, dtype=np.float32)
    kk = np.arange(128)
    c["tri01"] = (kk[None, :] >= kk[:, None]).astype(np.float32)
    c["trif"] = (kk[:, None] <= kk[None, :]).astype(np.float32)
    pos = np.arange(S)
    ksd = np.zeros((10, 2, S), np.float32)
    for kb in range(8):
        ksd[kb, :, :] = (pos // 256 == kb).astype(np.float32)[None, :]
    ksd[8:10] = 1.0
    c["kside"] = ksd
    mt = np.zeros((128, NT, 8), np.float32)
    for t in range(NT):
        own = t // 2
        for kb in range(8):
            mt[:, t, kb] = 0.0 if kb < own else (1e30 if kb == own else -1e30)
    c["mtable"] = mt.reshape(128, NT * 8)
    slopes = 2.0 ** (-8.0 * (np.arange(8) + 1.0) / 8)
    tokpos = (np.arange(NT)[None, :] * 128 + np.arange(128)[:, None]).astype(np.float64)
    hi = np.floor(tokpos / 256) * 256
    lo = tokpos - hi
    al = np.zeros((128, NT, 8, 2), np.float32)
    kb_ = np.zeros((128, NT, 8), np.float32)
    for h in range(8):
        al[:, :, h, 0] = -slopes[h] * hi
        al[:, :, h, 1] = -slopes[h] * lo
        kb_[:, :, h] = slopes[h] * tokpos
    c["alibi_q"] = al.reshape(128, NT * 8 * 2)
    c["alibi_k"] = kb_.reshape(128, NT * 8)
    return c


CONST_SHAPES = dict(ident=(128, 128), tri01=(128, 128), trif=(128, 128), kside=(10, 2, S),
                    mtable=(128, 128), alibi_q=(128, 256), alibi_k=(128, 128))


def build(dbg=False):
    nc = bass.Bass("TRN2", target_bir_lowering=False)

    def din(name, shape):
        return nc.dram_tensor(name, list(shape), F32, kind="ExternalInput").ap()

    x_in = din("x", (NSEQ, S, D))
    w_in = din("w_in", (DEPTH, D, INW))
    w_br = din("w_branch", (DEPTH, 3, 512, D))
    w_out = din("w_out", (DEPTH, D, D))
    gfull_d = din("gfull", (128, DEPTH * 8 * 128))
    fgrep_d = din("fgrep", (128, D))
    fbf_d = din("fbf_rep", (128, DEPTH * 128))
    bif_d = din("bif_rep", (128, DEPTH * 128))
    cw_d = din("cw", (128, DEPTH * 16))
    hg_d = din("hg", (128, DEPTH * 4))
    cd = {k: din("c_" + k, v) for k, v in CONST_SHAPES.items()}
    out_d = nc.dram_tensor("out", [NSEQ, S, D], F32, kind="ExternalOutput").ap()
    xs_d = nc.dram_tensor("xs_scratch", [NSEQ, S, D], F32, kind="Internal").ap()
    dbg_d = {}
    if dbg:
        for nm in ("dbg_hnT",):
            dbg_d[nm] = nc.dram_tensor(nm, [128, 8 * S], F32, kind="ExternalOutput").ap()
        for nm in ("dbg_ya", "dbg_yb", "dbg_yc"):
            dbg_d[nm] = nc.dram_tensor(nm, [128, 4 * S], F32, kind="ExternalOutput").ap()
        dbg_d["dbg_x1"] = nc.dram_tensor("dbg_x1", [S, D], F32, kind="ExternalOutput").ap()

    TOTAL = 212800
    big = nc.alloc_sbuf_tensor("big", [128, TOTAL], U8)
    cur = [0]

    def alloc(nbytes):
        o = cur[0]
        cur[0] = o + ((nbytes + 63) // 64) * 64
        assert cur[0] <= TOTAL, cur[0]
        return o

    def view(off, n, dt):
        sz = 4 if dt == F32 else 2
        return big[:, off:off + n * sz].bitcast(dt)

    def tl(n, dt):
        sz = 4 if dt == F32 else 2
        return view(alloc(n * sz), n, dt)

    hnT = tl(8 * S, BF16).rearrange("p (c n) -> p c n", c=8)
    yT = [tl(4 * S, BF16).rearrange("p (c n) -> p c n", c=4) for _ in range(3)]
    ident = tl(128, BF16)
    tri01 = tl(128, BF16)
    onesb = tl(128, BF16)
    o128b = tl(128, BF16)
    trif = tl(128, F32)
    onesf = tl(128, F32)
    gfull = tl(DEPTH * 8 * 128, BF16).rearrange("p (l c j) -> p l c j", l=DEPTH, c=8)
    fgrep = tl(D, F32)
    fbf = tl(DEPTH * 128, F32).rearrange("p (l n) -> p l n", l=DEPTH)
    bif = tl(DEPTH * 128, F32).rearrange("p (l n) -> p l n", l=DEPTH)
    cw = tl(DEPTH * 16, F32).rearrange("p (l c j) -> p l c j", l=DEPTH, c=4)
    hg = tl(DEPTH * 4, F32).rearrange("p (l h) -> p l h", l=DEPTH)
    mtable = tl(128, F32)
    alibi_k = tl(128, F32)
    epsc = tl(1, F32)
    stage0 = cur[0]

    ps = [nc.alloc_psum_tensor("ps%d" % i, [128, 512], F32)[:, :] for i in range(8)]
    bps = [Buf("ps%d" % i) for i in range(8)]
    psb = [ps[7], ps[6]]
    bpsb = [bps[7], bps[6]]

    P = Prog(nc)
    b_hnT = [Buf("hnT%d" % i) for i in range(NCH)]
    b_yT = [[[Buf("y") for _ in range(NCH)] for _ in range(4)] for _ in range(3)]
    b_const = Buf("const")

    def ld(dst, src):
        q = "sync" if dst.dtype == src.dtype else "gpsimd"
        P.op(q, lambda e: e.dma_start(out=dst, in_=src), writes=[b_const], dma_ch="const_" + q)

    ld(ident, cd["ident"])
    ld(tri01, cd["tri01"])
    ld(trif, cd["trif"])
    ld(gfull.rearrange("p l c j -> p (l c j)"), gfull_d)
    ld(fgrep, fgrep_d)
    ld(fbf.rearrange("p l n -> p (l n)"), fbf_d)
    ld(bif.rearrange("p l n -> p (l n)"), bif_d)
    ld(cw.rearrange("p l c j -> p (l c j)"), cw_d)
    ld(hg.rearrange("p l h -> p (l h)"), hg_d)
    ld(mtable, cd["mtable"])
    ld(alibi_k, cd["alibi_k"])
    P.op("gpsimd", lambda e: e.memset(onesb, 1.0), writes=[b_const])
    P.op("gpsimd", lambda e: e.memset(o128b, 1.0 / 128.0), writes=[b_const])
    P.op("gpsimd", lambda e: e.memset(onesf, 1.0), writes=[b_const])
    P.op("gpsimd", lambda e: e.memset(epsc, EPS), writes=[b_const])
    P.barrier()

    RC = [b_const]
    psrr = [0]

    def proj_fm(wt, bw, j0, chunk, pidx):
        for c in range(8):
            P.op("tensor", lambda e, c=c: e.matmul(ps[pidx][:, :], lhsT=wt[:, c, j0:j0 + 128],
                                                   rhs=hnT[:, c, chunk * 512:(chunk + 1) * 512],
                                                   start=(c == 0), stop=(c == 7)),
                 reads=[bw, b_hnT[chunk]], writes=[bps[pidx]])

    def wload(wt, bw, ch, l, cols, eng="gpsimd"):
        src = w_in[l].rearrange("(c p) n -> p c n", p=128)

        def fn(e):
            return [e.dma_start(out=wt[:, :, d0:d0 + n], in_=src[:, :, c0:c0 + n]) for (c0, n, d0) in cols]
        P.op(eng, fn, writes=[bw], dma_ch=ch, dma_n=len(cols))

    def norm_stage(s, l):
        cur[0] = stage0
        xt = [tl(D, F32) for _ in range(2)]
        hnb = [tl(D, BF16) for _ in range(2)]
        ss = tl(NT, F32)
        rstd = tl(NT, F32)
        bxt = [Buf("xt0"), Buf("xt1")]
        bhnb = [Buf("hnb0"), Buf("hnb1")]
        bss = Buf("ss")
        src = x_in[s] if l == 0 else xs_d[s]
        import os
        ncut = int(os.environ.get("MK_NCUT", "99"))
        for t in range(NT):
            i = t % 2
            P.op("sync", lambda e, i=i, t=t: e.dma_start(out=xt[i], in_=src[t * 128:(t + 1) * 128, :]),
                 writes=[bxt[i]], dma_ch="xt%d" % i)
            if ncut < 1:
                continue
            P.op("scalar", lambda e, i=i, t=t: e.activation(out=hnb[i], in_=xt[i], func=AF.Square,
                                                            accum_out=ss[:, t:t + 1]),
                 reads=[bxt[i]], writes=[bhnb[i], bss])
            if ncut < 2:
                continue
            P.op("scalar", lambda e, t=t: e.activation(out=rstd[:, t:t + 1], in_=ss[:, t:t + 1], func=AF.Ln,
                                                       bias=epsc, scale=1.0 / D),
                 reads=[bss] + RC, writes=[bss])
            P.op("scalar", lambda e, t=t: e.activation(out=rstd[:, t:t + 1], in_=rstd[:, t:t + 1], func=AF.Exp,
                                                       scale=-0.5),
                 reads=[bss], writes=[bss])
            if ncut < 3:
                continue
            P.op("scalar", lambda e, i=i, t=t: e.activation(out=hnb[i], in_=xt[i], func=AF.Copy,
                                                            scale=rstd[:, t:t + 1]),
                 reads=[bxt[i], bss], writes=[bhnb[i]])
            if ncut < 4:
                continue
            for hf in range(2):
                for cc in range(4):
                    c = hf * 4 + cc
                    P.op("tensor", lambda e, i=i, c=c, cc=cc, hf=hf: e.matmul(
                        psb[hf][:, cc * 128:(cc + 1) * 128], lhsT=hnb[i][:, c * 128:(c + 1) * 128], rhs=ident,
                        start=True, stop=True),
                        reads=[bhnb[i]] + RC, writes=[bpsb[hf]])
                eng = "vector"
                if ncut < 5:
                    continue
                P.op(eng, lambda e, hf=hf, t=t: e.tensor_tensor(
                    out=hnT[:, hf * 4:(hf + 1) * 4, t * 128:(t + 1) * 128],
                    in0=psb[hf].rearrange("p (c j) -> p c j", c=4),
                    in1=gfull[:, l, hf * 4:(hf + 1) * 4, :], op=ALU.mult),
                    reads=[bpsb[hf]] + RC, writes=[b_hnT[t // 4]])
        P.barrier()

    def attn_stage(s, l, br):
        cur[0] = stage0
        pre = "a_" if br == 0 else "b_"
        qaug = tl(2 * S, BF16).rearrange("p (h n) -> p h n", h=2)
        kaug = tl(2 * S, BF16).rearrange("p (h n) -> p h n", h=2)
        zs = tl(S, BF16)
        vaug = tl(NT * 768, BF16).rearrange("p (t n) -> p t n", t=NT)
        wv = tl(8 * 512, BF16).rearrange("p (c n) -> p c n", c=8)
        wf = tl(8 * 8, BF16).rearrange("p (c n) -> p c n", c=8)
        wp = [tl(8 * 384, BF16).rearrange("p (c n) -> p c n", c=8) for _ in range(2)]
        pt = [tl(512, BF16) for _ in range(6)]
        ext = tl(NT * 8 * 10, BF16).rearrange("p (t h e) -> p t h e", t=NT, h=8)
        gk = tl(128, F32)
        u1 = tl(128, F32)
        u2 = tl(128, F32)
        gp = tl(128, F32)
        srt = tl(128, F32)
        thr = tl(NT, F32)
        km = tl(16, F32)
        kmb = tl(16, BF16)
        rr = [tl(512, F32) for _ in range(2)]
        tm = [tl(512, F32) for _ in range(2)]
        b_q = [[Buf("q") for _ in range(NCH)] for _ in range(2)]
        b_k = [[Buf("k") for _ in range(NCH)] for _ in range(2)]
        b_z = [Buf("z") for _ in range(NCH)]
        b_v = [Buf("v") for _ in range(NT)]
        b_wv, b_wf = Buf("wv"), Buf("wf")
        b_wp = [Buf("wp0"), Buf("wp1")]
        b_pt = [Buf("pt") for _ in range(6)]
        b_ext = Buf("ext")
        b_sm = Buf("small")
        b_km = Buf("km")
        b_rr = [Buf("rr0"), Buf("rr1")]
        b_tm = [Buf("tm0"), Buf("tm1")]
        yt = yT[br]
        byt = b_yT[br]

        b_kc = Buf("kconst")
        P.op("gpsimd", lambda e: e.dma_start(out=kaug[64:74, :, :], in_=cd["kside"]), writes=[b_kc], dma_ch="kconst")
        P.op("gpsimd", lambda e: e.memset(vaug.rearrange("p t n -> p (t n)"), 1.0), writes=b_v)
        P.op("gpsimd", lambda e: e.memset(ext.rearrange("p t h e -> p (t h e)"), 0.0), writes=[b_ext])
        if br == 1:
            alq = tl(256, BF16)
            b_alq = Buf("alq")
            P.op("gpsimd", lambda e: e.dma_start(out=alq, in_=cd["alibi_q"]), writes=[b_alq], dma_ch="alq")
            P.op("gpsimd", lambda e: e.tensor_copy(out=ext[:, :, :, 8:10],
                                                   in_=alq.rearrange("p (t h e) -> p t h e", t=NT, h=8)),
                 reads=[b_alq], writes=[b_ext])
        wload(wv, b_wv, "wv", l, [(OFF[pre + "v"], 512, 0)])
        if br == 0:
            wload(wf, b_wf, "wf", l, [(OFF["a_f"], 8, 0)])

        def load_pair(j):
            wload(wp[j % 2], b_wp[j % 2], "wp%d" % (j % 2), l,
                  [(OFF[pre + "q"] + j * 128, 128, 0), (OFF[pre + "k"] + j * 128, 128, 128),
                   (OFF[pre + "z"] + j * 128, 128, 256)])
        load_pair(0)

        for t in range(NT):
            pi = t % 2
            for c in range(8):
                P.op("tensor", lambda e, c=c, t=t, pi=pi: e.matmul(
                    ps[pi][:, :], lhsT=hnT[:, c, t * 128:(t + 1) * 128], rhs=wv[:, c, :],
                    start=(c == 0), stop=(c == 7)), reads=[b_wv, b_hnT[t // 4]], writes=[bps[pi]])
            vv = vaug[:, t, :].rearrange("p (j n) -> p j n", j=4)
            pv = ps[pi].rearrange("p (j hh d) -> p j hh d", j=4, hh=2)
            P.op("scalar", lambda e, vv=vv, pv=pv: e.activation(out=vv[:, :, 0:64], in_=pv[:, :, 0, :], func=AF.Copy),
                 reads=[bps[pi]], writes=[b_v[t]])
            P.op("vector", lambda e, vv=vv, pv=pv: e.tensor_copy(out=vv[:, :, 128:192], in_=pv[:, :, 1, :]),
                 reads=[bps[pi]], writes=[b_v[t]])

        if br == 0:
            for t in range(NT):
                for c in range(8):
                    P.op("tensor", lambda e, c=c, t=t: e.matmul(
                        ps[6][:, t * 8:(t + 1) * 8], lhsT=hnT[:, c, t * 128:(t + 1) * 128], rhs=wf[:, c, :],
                        start=(c == 0), stop=(c == 7)), reads=[b_wf, b_hnT[t // 4]], writes=[bps[6]])
            P.op("vector", lambda e: e.tensor_tensor(out=u1, in0=ps[6][:, 0:128], in1=fbf[:, l, :], op=ALU.add),
                 reads=[bps[6]] + RC, writes=[b_sm])
            P.op("scalar", lambda e: e.activation(out=u2, in_=u1, func=AF.Exp, scale=-1.0), reads=[b_sm], writes=[b_sm])
            P.op("vector", lambda e: e.tensor_scalar_add(out=u2, in0=u2, scalar1=1.0), reads=[b_sm], writes=[b_sm])
            P.op("scalar", lambda e: e.activation(out=u1, in_=u2, func=AF.Ln), reads=[b_sm], writes=[b_sm])
            for t in range(NT):
                for tp in range(t + 1):
                    P.op("tensor", lambda e, t=t, tp=tp: e.matmul(
                        ps[5][:, t * 8:(t + 1) * 8], lhsT=(trif if tp == t else onesf),
                        rhs=u1[:, tp * 8:(tp + 1) * 8], start=(tp == 0), stop=(tp == t)),
                        reads=[b_sm] + RC, writes=[bps[5]])
            P.op("vector", lambda e: e.tensor_copy(out=gk, in_=ps[5][:, 0:128]), reads=[bps[5]], writes=[b_sm])
            gv = gk.rearrange("p (t h) -> p t h", t=NT)
            P.op("vector", lambda e: e.tensor_scalar(out=ext[:, :, :, 8], in0=gv, scalar1=-1.0, scalar2=None,
                                                     op0=ALU.mult), reads=[b_sm], writes=[b_ext])
            P.op("vector", lambda e: e.scalar_tensor_tensor(out=ext[:, :, :, 9], in0=gv, scalar=-1.0,
                                                            in1=ext[:, :, :, 8], op0=ALU.mult, op1=ALU.subtract),
                 reads=[b_sm, b_ext], writes=[b_ext])
            biast = gk
        else:
            biast = alibi_k

        def attention(hh, h):
            j = h // 2
            lo, hi_ = (0, 64) if hh == 0 else (64, 128)
            llo = 64 if hh == 0 else 0
            vc0 = j * 192 + (0 if hh == 0 else 64)
            tiles = []
            for qc in range(NCH):
                for kt in range(4 * qc + 4):
                    tiles.append((qc, kt))
            pend = []
            pidx = [0]

            def issue_pv(item):
                qc, kt, c0, n, pti = item
                po = 4 + (qc % 2)
                P.op("tensor", lambda e: e.matmul(
                    ps[po][:, c0:c0 + n], lhsT=vaug[:, kt, vc0:vc0 + 128], rhs=pt[pti][:, 0:n],
                    start=(kt == 0), stop=(kt == 4 * qc + 3)),
                    reads=[b_v[kt], b_pt[pti]], writes=[bps[po]])
                if kt == 4 * qc + 3:
                    fin(qc, po)

            def fin(qc, po):
                ri = qc % 2
                P.op("vector", lambda e: e.reciprocal(out=rr[ri][lo:hi_, :], in_=ps[po][llo:llo + 64, :]),
                     reads=[bps[po]], writes=[b_rr[ri]])
                P.op("vector", lambda e: e.tensor_tensor(out=tm[ri][lo:hi_, :], in0=ps[po][lo:hi_, :],
                                                         in1=rr[ri][lo:hi_, :], op=ALU.mult),
                     reads=[bps[po], b_rr[ri]], writes=[b_tm[ri]])
                P.op("gpsimd", lambda e: e.tensor_tensor(out=yt[lo:hi_, j, qc * 512:(qc + 1) * 512],
                                                         in0=tm[ri][lo:hi_, :], in1=zs[lo:hi_, qc * 512:(qc + 1) * 512],
                                                         op=ALU.mult),
                     reads=[b_tm[ri], b_z[qc]], writes=[byt[j][qc]])

            for (qc, kt) in tiles:
                d = kt - 4 * qc
                c0 = 0 if d < 0 else d * 128
                n = 512 - c0
                si = pidx[0] % 4
                pti = pidx[0] % 6
                pidx[0] += 1
                q0 = qc * 512 + c0
                P.op("tensor", lambda e, kt=kt, q0=q0, n=n, si=si: e.matmul(
                    ps[si][:, 0:n], lhsT=kaug[0:74, hh, kt * 128:(kt + 1) * 128], rhs=qaug[0:74, hh, q0:q0 + n],
                    start=True, stop=True),
                    reads=[b_k[hh][kt // 4], b_q[hh][qc], b_kc], writes=[bps[si]])
                P.op("scalar", lambda e, kt=kt, n=n, si=si, pti=pti: e.activation(
                    out=pt[pti][:, 0:n], in_=ps[si][:, 0:n], func=AF.Exp, bias=biast[:, kt * 8 + h:kt * 8 + h + 1]),
                    reads=[bps[si], b_sm] + RC, writes=[b_pt[pti]])
                if d >= 0:
                    P.op("gpsimd", lambda e, pti=pti: e.tensor_tensor(out=pt[pti][:, 0:128], in0=pt[pti][:, 0:128],
                                                                      in1=tri01, op=ALU.mult),
                         reads=[b_pt[pti]] + RC, writes=[b_pt[pti]])
                pend.append((qc, kt, c0, n, pti))
                if len(pend) > 3:
                    issue_pv(pend.pop(0))
            while pend:
                issue_pv(pend.pop(0))

        for j in range(4):
            w = wp[j % 2]
            bw = b_wp[j % 2]
            if j + 1 < 4:
                load_pair(j + 1)
            for chunk in range(NCH):
                cs = slice(chunk * 512, (chunk + 1) * 512)
                pi = chunk % 2
                proj_fm(w, bw, 0, chunk, pi)
                P.op("scalar", lambda e, cs=cs, pi=pi: e.activation(out=qaug[0:64, 0, cs], in_=ps[pi][0:64, :],
                                                                    func=AF.Copy, scale=0.125),
                     reads=[bps[pi]], writes=[b_q[0][chunk]])
                P.op("vector", lambda e, cs=cs, pi=pi: e.tensor_scalar(out=qaug[0:64, 1, cs], in0=ps[pi][64:128, :],
                                                                       scalar1=0.125, scalar2=None, op0=ALU.mult),
                     reads=[bps[pi]], writes=[b_q[1][chunk]])
            for chunk in range(NCH):
                cs = slice(chunk * 512, (chunk + 1) * 512)
                pi = chunk % 2
                proj_fm(w, bw, 128, chunk, pi)
                P.op("scalar", lambda e, cs=cs, pi=pi: e.activation(out=kaug[0:64, 0, cs], in_=ps[pi][0:64, :],
                                                                    func=AF.Copy),
                     reads=[bps[pi]], writes=[b_k[0][chunk]])
                P.op("vector", lambda e, cs=cs, pi=pi: e.tensor_copy(out=kaug[0:64, 1, cs], in_=ps[pi][64:128, :]),
                     reads=[bps[pi]], writes=[b_k[1][chunk]])
            for chunk in range(NCH):
                cs = slice(chunk * 512, (chunk + 1) * 512)
                pi = chunk % 2
                proj_fm(w, bw, 256, chunk, pi)
                P.op("scalar", lambda e, cs=cs, pi=pi: e.activation(out=zs[:, cs], in_=ps[pi][:, :], func=AF.Silu),
                     reads=[bps[pi]], writes=[b_z[chunk]])
            for hh in range(2):
                h = 2 * j + hh
                if br == 1:
                    P.op("vector", lambda e, hh=hh: e.tensor_reduce(
                        out=km[0:64, hh * 8:(hh + 1) * 8],
                        in_=kaug[0:64, hh, :].rearrange("p (b k) -> p b k", k=256), axis=AX.X, op=ALU.add),
                        reads=b_k[hh], writes=[b_km])
                    P.op("vector", lambda e, hh=hh: e.tensor_copy(out=kmb[0:64, hh * 8:(hh + 1) * 8],
                                                                  in_=km[0:64, hh * 8:(hh + 1) * 8]),
                         reads=[b_km], writes=[b_km])
                    for t in range(NT):
                        P.op("tensor", lambda e, t=t, hh=hh: e.matmul(
                            ps[6][:, t * 8:(t + 1) * 8], lhsT=qaug[0:64, hh, t * 128:(t + 1) * 128],
                            rhs=kmb[0:64, hh * 8:(hh + 1) * 8], start=True, stop=True),
                            reads=[b_q[hh][t // 4], b_km], writes=[bps[6]])
                    P.op("vector", lambda e: e.tensor_tensor(out=gp, in0=ps[6][:, 0:128], in1=mtable, op=ALU.add),
                         reads=[bps[6]] + RC, writes=[b_sm])
                    for t in range(NT):
                        P.op("vector", lambda e, t=t: e.max(out=srt[:, t * 8:(t + 1) * 8], in_=gp[:, t * 8:(t + 1) * 8]),
                             reads=[b_sm], writes=[b_sm])
                    P.op("vector", lambda e: e.tensor_scalar(
                        out=thr, in0=srt.rearrange("p (t k) -> p t k", k=8)[:, :, 3], scalar1=-1e29, scalar2=None,
                        op0=ALU.max), reads=[b_sm], writes=[b_sm])
                    for t in range(NT):
                        P.op("vector", lambda e, t=t, h=h: e.tensor_scalar(
                            out=ext[:, t, h, 0:8], in0=gp[:, t * 8:(t + 1) * 8], scalar1=thr[:, t:t + 1],
                            scalar2=-BIG, op0=ALU.is_lt, op1=ALU.mult), reads=[b_sm], writes=[b_ext])
                for chunk in range(NCH):
                    pb = chunk % 2
                    for tt in range(4):
                        t = chunk * 4 + tt
                        P.op("tensor", lambda e, t=t, tt=tt, pb=pb, h=h: e.matmul(
                            psb[pb][0:10, tt * 128:(tt + 1) * 128], lhsT=ext[:, t, h, :], rhs=ident,
                            start=True, stop=True),
                            reads=[b_ext] + RC, writes=[bpsb[pb]])
                    P.op("vector", lambda e, chunk=chunk, pb=pb, hh=hh: e.tensor_copy(
                        out=qaug[64:74, hh, chunk * 512:(chunk + 1) * 512], in_=psb[pb][0:10, :]),
                        reads=[bpsb[pb]], writes=[b_q[hh][chunk]])
                attention(hh, h)
        P.barrier()

    def mlstm_stage(s, l):
        cur[0] = stage0
        qkT = tl(4 * S, BF16).rearrange("p (c n) -> p c n", c=4)
        cst0 = tl(S + 4, F32)
        acc0 = tl(S, F32)
        vC = tl(NT * 512, BF16).rearrange("p (t n) -> p t n", t=NT)
        oT = tl(S, BF16)
        zT = tl(S, BF16)
        faug = tl(2 * S, BF16).rearrange("p (h n) -> p h n", h=2)
        wv = tl(8 * 512, BF16).rearrange("p (c n) -> p c n", c=8)
        wf = tl(8 * 8, BF16).rearrange("p (c n) -> p c n", c=8)
        wq = [tl(8 * 128, BF16).rearrange("p (c n) -> p c n", c=8) for _ in range(2)]
        wp = [tl(8 * 256, BF16).rearrange("p (c n) -> p c n", c=8) for _ in range(2)]
        ovl = cur[0]
        dt_ = [tl(512, F32) for _ in range(3)]
        st_ = [tl(512, BF16) for _ in range(6)]
        fa = [tl(512, F32) for _ in range(2)]
        fb = [tl(512, F32) for _ in range(2)]
        fsq = [tl(512, BF16) for _ in range(2)]
        assert cur[0] - ovl >= 8256 + 8192
        cst = [cst0, view(ovl, S + 4, F32)]
        acc = [acc0, view(ovl + 8256, S, F32)]
        ext = tl(NT * 4 * 4, BF16).rearrange("p (t h e) -> p t h e", t=NT, h=4)
        u1 = tl(128, F32)
        spf = tl(64, F32)
        e1 = tl(64, F32)
        gf = tl(64, F32)
        bias_c = tl(64, F32)
        r1 = tl(64, F32)
        b_qk = [[Buf("qk") for _ in range(NCH)] for _ in range(4)]
        b_cst, b_acc = [Buf("cst0"), Buf("cst1")], [Buf("acc0"), Buf("acc1")]
        b_v = [Buf("v") for _ in range(NT)]
        b_o = [Buf("o") for _ in range(NCH)]
        b_z = [Buf("z") for _ in range(NCH)]
        b_fa = [[Buf("faug") for _ in range(NCH)] for _ in range(2)]
        b_wv, b_wf = Buf("wv"), Buf("wf")
        b_wq = [Buf("wq0"), Buf("wq1")]
        b_wp = [Buf("wp0"), Buf("wp1")]
        b_dt = [Buf("dt") for _ in range(3)]
        b_st = [Buf("st") for _ in range(6)]
        b_ext, b_sm = Buf("ext"), Buf("small")
        b_f = [[Buf("fa"), Buf("fb"), Buf("fsq")] for _ in range(2)]
        yt = yT[2]
        byt = b_yT[2]

        for bi_ in range(2):
            P.op("gpsimd", lambda e, bi_=bi_: e.memset(cst[bi_][:, 0:4], 0.0), writes=[b_cst[bi_]])
        P.op("gpsimd", lambda e: e.memset(faug.rearrange("p h n -> p (h n)"), 0.0), writes=[x for y in b_fa for x in y])
        wload(wv, b_wv, "wv", l, [(OFF["c_v"], 512, 0)])
        wload(wf, b_wf, "wf", l, [(OFF["c_if"], 8, 0)])
        wload(wq[0], b_wq[0], "wq0", l, [(OFF["c_qk"], 128, 0)])

        for t in range(NT):
            pi = t % 2
            for c in range(8):
                P.op("tensor", lambda e, c=c, t=t, pi=pi: e.matmul(
                    ps[pi][:, :], lhsT=hnT[:, c, t * 128:(t + 1) * 128], rhs=wv[:, c, :],
                    start=(c == 0), stop=(c == 7)), reads=[b_wv, b_hnT[t // 4]], writes=[bps[pi]])
            eng = "scalar" if t % 2 == 0 else "vector"
            if eng == "scalar":
                P.op("scalar", lambda e, t=t, pi=pi: e.activation(out=vC[:, t, :], in_=ps[pi][:, :], func=AF.Copy),
                     reads=[bps[pi]], writes=[b_v[t]])
            else:
                P.op("vector", lambda e, t=t, pi=pi: e.tensor_copy(out=vC[:, t, :], in_=ps[pi][:, :]),
                     reads=[bps[pi]], writes=[b_v[t]])

        for t in range(NT):
            for c in range(8):
                P.op("tensor", lambda e, c=c, t=t: e.matmul(
                    ps[6][:, t * 8:(t + 1) * 8], lhsT=hnT[:, c, t * 128:(t + 1) * 128], rhs=wf[:, c, :],
                    start=(c == 0), stop=(c == 7)), reads=[b_wf, b_hnT[t // 4]], writes=[bps[6]])
        P.op("vector", lambda e: e.tensor_tensor(out=u1, in0=ps[6][:, 0:128], in1=bif[:, l, :], op=ALU.add),
             reads=[bps[6]] + RC, writes=[b_sm])
        u1v = u1.rearrange("p (t g) -> p t g", t=NT)
        e1v = e1.rearrange("p (t g) -> p t g", t=NT)
        P.op("scalar", lambda e: e.activation(out=e1v, in_=u1v[:, :, 4:8], func=AF.Exp, scale=-1.0),
             reads=[b_sm], writes=[b_sm])
        P.op("vector", lambda e: e.tensor_scalar_add(out=e1, in0=e1, scalar1=1.0), reads=[b_sm], writes=[b_sm])
        P.op("scalar", lambda e: e.activation(out=spf, in_=e1, func=AF.Ln), reads=[b_sm], writes=[b_sm])
        for t in range(NT):
            for tp in range(t + 1):
                P.op("tensor", lambda e, t=t, tp=tp: e.matmul(
                    ps[5][:, t * 4:(t + 1) * 4], lhsT=(trif if tp == t else onesf),
                    rhs=spf[:, tp * 4:(tp + 1) * 4], start=(tp == 0), stop=(tp == t)),
                    reads=[b_sm] + RC, writes=[bps[5]])
        P.op("vector", lambda e: e.tensor_copy(out=gf, in_=ps[5][:, 0:64]), reads=[bps[5]], writes=[b_sm])
        gfv = gf.rearrange("p (t h) -> p t h", t=NT)
        P.op("vector", lambda e: e.tensor_tensor(out=bias_c.rearrange("p (t h) -> p t h", t=NT), in0=u1v[:, :, 0:4],
                                                 in1=gfv, op=ALU.add), reads=[b_sm], writes=[b_sm])
        r1v = r1.rearrange("p (t h) -> p t h", t=NT)
        P.op("vector", lambda e: e.tensor_scalar(out=ext[:, :, :, 0], in0=gfv, scalar1=-1.0, scalar2=None, op0=ALU.mult),
             reads=[b_sm], writes=[b_ext])
        P.op("vector", lambda e: e.scalar_tensor_tensor(out=r1v, in0=gfv, scalar=-1.0, in1=ext[:, :, :, 0],
                                                        op0=ALU.mult, op1=ALU.subtract),
             reads=[b_sm, b_ext], writes=[b_sm])
        P.op("vector", lambda e: e.tensor_copy(out=ext[:, :, :, 1], in_=r1v), reads=[b_sm, b_ext], writes=[b_ext])
        P.op("vector", lambda e: e.tensor_tensor(out=ext[:, :, :, 2], in0=r1v, in1=ext[:, :, :, 1], op=ALU.subtract),
             reads=[b_sm, b_ext], writes=[b_ext])
        P.op("vector", lambda e: e.memset(ext[:, :, :, 3], 0.0), reads=[b_ext], writes=[b_ext])
        for h in range(4):
            for chunk in range(NCH):
                pb = chunk % 2
                for tt in range(4):
                    t = chunk * 4 + tt
                    P.op("tensor", lambda e, t=t, tt=tt, pb=pb, h=h: e.matmul(
                        psb[pb][0:4, tt * 128:(tt + 1) * 128], lhsT=ext[:, t, h, :], rhs=ident,
                        start=True, stop=True),
                        reads=[b_ext] + RC, writes=[bpsb[pb]])
                r0 = 32 * (h % 2)
                P.op("vector", lambda e, chunk=chunk, pb=pb, h=h, r0=r0: e.tensor_copy(
                    out=faug[r0:r0 + 4, h // 2, chunk * 512:(chunk + 1) * 512], in_=psb[pb][0:4, :]),
                    reads=[bpsb[pb]], writes=[b_fa[h // 2][chunk]])

        for cc in range(4):
            w = wq[cc % 2]
            bw = b_wq[cc % 2]
            cb = cc % 2
            if cc + 1 < 4:
                wload(wq[(cc + 1) % 2], b_wq[(cc + 1) % 2], "wq%d" % ((cc + 1) % 2), l,
                      [(OFF["c_qk"] + (cc + 1) * 128, 128, 0)])
            for chunk in range(NCH):
                pi = chunk % 2
                proj_fm(w, bw, 0, chunk, pi)
                dst = cst[cb][:, 4 + chunk * 512:4 + (chunk + 1) * 512]
                if chunk % 2 == 0:
                    P.op("scalar", lambda e, dst=dst, pi=pi: e.activation(out=dst, in_=ps[pi][:, :], func=AF.Copy),
                         reads=[bps[pi]], writes=[b_cst[cb]])
                else:
                    P.op("vector", lambda e, dst=dst, pi=pi: e.tensor_copy(out=dst, in_=ps[pi][:, :]),
                         reads=[bps[pi]], writes=[b_cst[cb]])
            P.op("scalar", lambda e, cc=cc, cb=cb: e.activation(out=acc[cb], in_=cst[cb][:, 4:4 + S], func=AF.Copy,
                                                                scale=cw[:, l, cc, 3:4]),
                 reads=[b_cst[cb]] + RC, writes=[b_acc[cb]])
            for jj in (2, 1, 0):
                sh = 3 - jj
                P.op("vector", lambda e, cc=cc, jj=jj, sh=sh, cb=cb: e.scalar_tensor_tensor(
                    out=acc[cb], in0=cst[cb][:, 4 - sh:4 - sh + S], scalar=cw[:, l, cc, jj:jj + 1], in1=acc[cb],
                    op0=ALU.mult, op1=ALU.add), reads=[b_cst[cb], b_acc[cb]] + RC, writes=[b_acc[cb]])
            for chunk in range(NCH):
                P.op("scalar", lambda e, cc=cc, chunk=chunk, cb=cb: e.activation(
                    out=qkT[:, cc, chunk * 512:(chunk + 1) * 512], in_=acc[cb][:, chunk * 512:(chunk + 1) * 512],
                    func=AF.Silu), reads=[b_acc[cb]], writes=[b_qk[cc][chunk]])
        P.barrier()

        def load_head(h):
            wload(wp[h % 2], b_wp[h % 2], "wp%d" % (h % 2), l,
                  [(OFF["c_o"] + h * 128, 128, 0), (OFF["c_z"] + h * 128, 128, 128)])
        load_head(0)

        def attention(h):
            pb0 = 64 * (h % 2)
            qch = h // 2
            kch = 2 + h // 2
            r0 = 32 * (h % 2)
            fs = h // 2
            tiles = [(qc, kt) for qc in range(NCH) for kt in range(4 * qc + 4)]
            pend = []
            cnt = [0]

            def issue_pv(item):
                qc, kt, c0, n, sti = item
                P.op("tensor", lambda e: e.matmul(ps[4][:, c0:c0 + n], lhsT=vC[:, kt, h * 128:(h + 1) * 128],
                                                  rhs=st_[sti][:, 0:n], start=(kt == 0), stop=(kt == 4 * qc + 3)),
                     reads=[b_v[kt], b_st[sti]], writes=[bps[4]])
                P.op("tensor", lambda e: e.matmul(ps[5][:, c0:c0 + n], lhsT=onesb, rhs=st_[sti][:, 0:n],
                                                  start=(kt == 0), stop=(kt == 4 * qc + 3)),
                     reads=[b_st[sti]] + RC, writes=[bps[5]])
                if kt == 4 * qc + 3:
                    fin(qc)

            def fin(qc):
                fi = qc % 2
                cs = slice(qc * 512, (qc + 1) * 512)
                bf_a, bf_b, bf_s = b_f[fi]
                P.op("scalar", lambda e: e.activation(out=fa[fi], in_=ps[5][:, :], func=AF.Abs),
                     reads=[bps[5]], writes=[bf_a])
                P.op("vector", lambda e: e.tensor_scalar_max(out=fa[fi], in0=fa[fi], scalar1=1.0),
                     reads=[bf_a], writes=[bf_a])
                P.op("vector", lambda e: e.reciprocal(out=fa[fi], in_=fa[fi]), reads=[bf_a], writes=[bf_a])
                P.op("vector", lambda e: e.tensor_tensor(out=fb[fi], in0=ps[4][:, :], in1=fa[fi], op=ALU.mult),
                     reads=[bps[4], bf_a], writes=[bf_b])
                P.op("gpsimd", lambda e: e.tensor_tensor(out=fb[fi], in0=fb[fi], in1=oT[:, cs], op=ALU.mult),
                     reads=[bf_b, b_o[qc]], writes=[bf_b])
                P.op("scalar", lambda e: e.activation(out=fsq[fi], in_=fb[fi], func=AF.Square),
                     reads=[bf_b], writes=[bf_s])
                P.op("tensor", lambda e: e.matmul(ps[6][:, :], lhsT=o128b, rhs=fsq[fi], start=True, stop=True),
                     reads=[bf_s] + RC, writes=[bps[6]])
                P.op("scalar", lambda e: e.activation(out=fa[fi], in_=ps[6][:, :], func=AF.Ln, bias=epsc),
                     reads=[bps[6]] + RC, writes=[bf_a])
                P.op("scalar", lambda e: e.activation(out=fa[fi], in_=fa[fi], func=AF.Exp, scale=-0.5),
                     reads=[bf_a], writes=[bf_a])
                P.op("vector", lambda e: e.tensor_tensor(out=fb[fi], in0=fb[fi], in1=fa[fi], op=ALU.mult),
                     reads=[bf_a, bf_b], writes=[bf_b])
                P.op("vector", lambda e: e.scalar_tensor_tensor(out=yt[:, h, cs], in0=fb[fi], scalar=hg[:, l, h:h + 1],
                                                                in1=zT[:, cs], op0=ALU.mult, op1=ALU.mult),
                     reads=[bf_b, b_z[qc]] + RC, writes=[byt[h][qc]])

            for (qc, kt) in tiles:
                d = kt - 4 * qc
                c0 = 0 if d < 0 else d * 128
                n = 512 - c0
                k_ = cnt[0]
                cnt[0] += 1
                si = (2, 3, 6)[k_ % 3]
                ei = (0, 1, 7)[k_ % 3]
                di = k_ % 3
                sti = k_ % 6
                q0 = qc * 512 + c0
                P.op("tensor", lambda e, kt=kt, q0=q0, n=n, si=si: e.matmul(
                    ps[si][:, 0:n], lhsT=qkT[pb0:pb0 + 64, kch, kt * 128:(kt + 1) * 128],
                    rhs=qkT[pb0:pb0 + 64, qch, q0:q0 + n], start=True, stop=True),
                    reads=[b_qk[kch][kt // 4], b_qk[qch][qc]], writes=[bps[si]])
                P.op("tensor", lambda e, q0=q0, n=n, ei=ei: e.matmul(
                    ps[ei][:, 0:n], lhsT=onesb[r0:r0 + 4, :], rhs=faug[r0:r0 + 4, fs, q0:q0 + n],
                    start=True, stop=True), reads=[b_fa[fs][qc]] + RC, writes=[bps[ei]])
                P.op("scalar", lambda e, kt=kt, n=n, ei=ei, di=di: e.activation(
                    out=dt_[di][:, 0:n], in_=ps[ei][:, 0:n], func=AF.Exp, bias=bias_c[:, kt * 4 + h:kt * 4 + h + 1]),
                    reads=[bps[ei], b_sm], writes=[b_dt[di]])
                P.op("vector", lambda e, n=n, si=si, di=di, sti=sti: e.scalar_tensor_tensor(
                    out=st_[sti][:, 0:n], in0=ps[si][:, 0:n], scalar=0.125, in1=dt_[di][:, 0:n],
                    op0=ALU.mult, op1=ALU.mult), reads=[bps[si], b_dt[di]], writes=[b_st[sti]])
                if d >= 0:
                    P.op("gpsimd", lambda e, sti=sti: e.tensor_tensor(out=st_[sti][:, 0:128], in0=st_[sti][:, 0:128],
                                                                      in1=tri01, op=ALU.mult),
                         reads=[b_st[sti]] + RC, writes=[b_st[sti]])
                pend.append((qc, kt, c0, n, sti))
                if len(pend) > 3:
                    issue_pv(pend.pop(0))
            while pend:
                issue_pv(pend.pop(0))

        for h in range(4):
            w = wp[h % 2]
            bw = b_wp[h % 2]
            if h + 1 < 4:
                load_head(h + 1)
            for chunk in range(NCH):
                cs = slice(chunk * 512, (chunk + 1) * 512)
                pi = chunk % 2
                proj_fm(w, bw, 0, chunk, pi)
                P.op("scalar", lambda e, cs=cs, pi=pi: e.activation(out=oT[:, cs], in_=ps[pi][:, :], func=AF.Sigmoid),
                     reads=[bps[pi]], writes=[b_o[chunk]])
            for chunk in range(NCH):
                cs = slice(chunk * 512, (chunk + 1) * 512)
                pi = chunk % 2
                proj_fm(w, bw, 128, chunk, pi)
                P.op("scalar", lambda e, cs=cs, pi=pi: e.activation(out=zT[:, cs], in_=ps[pi][:, :], func=AF.Silu),
                     reads=[bps[pi]], writes=[b_z[chunk]])
            attention(h)
        P.barrier()

    def merge_stage(s, l, finals):
        cur[0] = stage0
        wo = tl(8 * D, BF16).rearrange("p (c n) -> p c n", c=8)
        wb = tl(12 * D, BF16).rearrange("p (c n) -> p c n", c=12)
        wg = [tl(8 * 512, BF16).rearrange("p (c n) -> p c n", c=8) for _ in range(2)]
        mbf = tl(8 * 1024, BF16).rearrange("p (c n) -> p c n", c=8)
        macc = [[tl(512, F32) for _ in range(2)] for _ in range(4)]
        gs = [tl(512, F32) for _ in range(2)]
        tmp = [tl(512, F32) for _ in range(2)]
        xr = [tl(D, F32) for _ in range(2)]
        jk = tl(D, BF16)
        ss = tl(NT, F32)
        b_wo, b_wb = Buf("wo"), Buf("wb")
        b_wg = [Buf("wg0"), Buf("wg1")]
        b_mbf = [Buf("mbf") for _ in range(8)]
        b_macc = [[Buf("macc") for _ in range(2)] for _ in range(4)]
        b_gs = [Buf("gs0"), Buf("gs1")]
        b_tmp = [Buf("tmp0"), Buf("tmp1")]
        b_xr = [Buf("xr0"), Buf("xr1")]
        b_jk, b_ss = Buf("jk"), Buf("ss")
        src = x_in[s] if l == 0 else xs_d[s]

        P.op("gpsimd", lambda e: [e.dma_start(out=wb[:, n * 4:(n + 1) * 4, :],
                                              in_=w_br[l, n].rearrange("(c p) d -> p c d", p=128)) for n in range(3)],
             writes=[b_wb], dma_ch="wb", dma_n=3)
        gcnt = [0]
        it = [0]
        first = True
        for half in range(2):
            for ftg in range(2):
                for n in range(3):
                    gi = gcnt[0] % 2
                    gcnt[0] += 1
                    wload(wg[gi], b_wg[gi], "wg%d" % gi, l, [(OFF["gates"] + n * 1024 + ftg * 512, 512, 0)])
                    if first:
                        first = False
                        P.op("gpsimd", lambda e: e.dma_start(out=wo, in_=w_out[l].rearrange("(c p) n -> p c n", p=128)),
                             writes=[b_wo], dma_ch="wo")
                    for fl in range(4):
                        ft = ftg * 4 + fl
                        for cc in range(2):
                            chunk = half * 2 + cc
                            cs = slice(chunk * 512, (chunk + 1) * 512)
                            k_ = it[0] % 2
                            it[0] += 1
                            pg = k_
                            pbk = 2 + k_
                            for c in range(8):
                                P.op("tensor", lambda e, c=c, gi=gi, cs=cs, pg=pg, fl=fl: e.matmul(
                                    ps[pg][:, :], lhsT=wg[gi][:, c, fl * 128:(fl + 1) * 128], rhs=hnT[:, c, cs],
                                    start=(c == 0), stop=(c == 7)),
                                    reads=[b_wg[gi], b_hnT[chunk]], writes=[bps[pg]])
                            for wc in range(4):
                                P.op("tensor", lambda e, wc=wc, n=n, ft=ft, cs=cs, pbk=pbk: e.matmul(
                                    ps[pbk][:, :], lhsT=wb[:, n * 4 + wc, ft * 128:(ft + 1) * 128], rhs=yT[n][:, wc, cs],
                                    start=(wc == 0), stop=(wc == 3)),
                                    reads=[b_wb, b_yT[n][wc][chunk]], writes=[bps[pbk]])
                            P.op("scalar", lambda e, k_=k_, pg=pg: e.activation(out=gs[k_], in_=ps[pg][:, :], func=AF.Sigmoid),
                                 reads=[bps[pg]], writes=[b_gs[k_]])
                            ma, bma = macc[fl][cc], b_macc[fl][cc]
                            if n == 0:
                                P.op("vector", lambda e, k_=k_, pbk=pbk, ma=ma: e.tensor_tensor(
                                    out=ma, in0=ps[pbk][:, :], in1=gs[k_], op=ALU.mult),
                                    reads=[bps[pbk], b_gs[k_]], writes=[bma])
                            else:
                                P.op("vector", lambda e, k_=k_, pbk=pbk: e.tensor_tensor(
                                    out=tmp[k_], in0=ps[pbk][:, :], in1=gs[k_], op=ALU.mult),
                                    reads=[bps[pbk], b_gs[k_]], writes=[b_tmp[k_]])
                                if n == 1:
                                    P.op("gpsimd", lambda e, k_=k_, ma=ma: e.tensor_tensor(out=ma, in0=ma, in1=tmp[k_], op=ALU.add),
                                         reads=[b_tmp[k_], bma], writes=[bma])
                                else:
                                    P.op("gpsimd", lambda e, k_=k_, ma=ma, ft=ft, cc=cc: e.tensor_tensor(
                                        out=mbf[:, ft, cc * 512:(cc + 1) * 512], in0=ma, in1=tmp[k_], op=ALU.add),
                                        reads=[b_tmp[k_], bma], writes=[b_mbf[ft]])
            for tt in range(8):
                t = half * 8 + tt
                i = tt % 2
                P.op("sync", lambda e, i=i, t=t: e.dma_start(out=xr[i], in_=src[t * 128:(t + 1) * 128, :]),
                     writes=[b_xr[i]], dma_ch="xr%d" % i)
                for hc in range(2):
                    po = 4 + hc
                    for fc in range(8):
                        P.op("tensor", lambda e, fc=fc, tt=tt, hc=hc, po=po: e.matmul(
                            ps[po][:, :], lhsT=mbf[:, fc, tt * 128:(tt + 1) * 128], rhs=wo[:, fc, hc * 512:(hc + 1) * 512],
                            start=(fc == 0), stop=(fc == 7)), reads=[b_wo, b_mbf[fc]], writes=[bps[po]])
                    P.op("vector", lambda e, i=i, hc=hc, po=po: e.tensor_tensor(
                        out=xr[i][:, hc * 512:(hc + 1) * 512], in0=ps[po][:, :], in1=xr[i][:, hc * 512:(hc + 1) * 512],
                        op=ALU.add), reads=[bps[po], b_xr[i]], writes=[b_xr[i]])
                if l == 0:
                    P.op("sync", lambda e, i=i, t=t: e.dma_start(out=xs_d[s, t * 128:(t + 1) * 128, :], in_=xr[i]),
                         reads=[b_xr[i]], dma_ch="st%d" % i)
                    if dbg and s == 0:
                        finals.append(P.op("sync", lambda e, i=i, t=t: e.dma_start(
                            out=dbg_d["dbg_x1"][t * 128:(t + 1) * 128, :], in_=xr[i]), reads=[b_xr[i]], dma_ch="dbgx"))
                else:
                    P.op("scalar", lambda e, i=i, t=t: e.activation(out=jk, in_=xr[i], func=AF.Square,
                                                                    accum_out=ss[:, t:t + 1]),
                         reads=[b_xr[i]], writes=[b_jk, b_ss])
                    P.op("scalar", lambda e, t=t: e.activation(out=ss[:, t:t + 1], in_=ss[:, t:t + 1], func=AF.Ln,
                                                               bias=epsc, scale=1.0 / D),
                         reads=[b_ss] + RC, writes=[b_ss])
                    P.op("scalar", lambda e, t=t: e.activation(out=ss[:, t:t + 1], in_=ss[:, t:t + 1], func=AF.Exp,
                                                               scale=-0.5),
                         reads=[b_ss], writes=[b_ss])
                    P.op("vector", lambda e, i=i, t=t: e.scalar_tensor_tensor(
                        out=xr[i], in0=xr[i], scalar=ss[:, t:t + 1], in1=fgrep, op0=ALU.mult, op1=ALU.mult),
                        reads=[b_xr[i], b_ss] + RC, writes=[b_xr[i]])
                    finals.append(P.op("sync", lambda e, i=i, t=t: e.dma_start(
                        out=out_d[s, t * 128:(t + 1) * 128, :], in_=xr[i]), reads=[b_xr[i]], dma_ch="st%d" % i))
        P.barrier()

    def dump(nm, src_ap, bufs, finals):
        finals.append(P.op("gpsimd", lambda e: e.dma_start(out=dbg_d[nm], in_=src_ap), reads=bufs, dma_ch="dbg_" + nm))

    finals = []
    import os
    limit = int(os.environ.get("MK_LIMIT", "1000"))
    nst = [0]

    def go(fn, *a):
        if nst[0] < limit:
            fn(*a)
        nst[0] += 1

    for s in range(NSEQ):
        for l in range(DEPTH):
            go(norm_stage, s, l)
            if dbg and s == 0 and l == 0:
                dump("dbg_hnT", hnT.rearrange("p c n -> p (c n)"), b_hnT, finals)
                P.barrier()
            go(attn_stage, s, l, 0)
            go(attn_stage, s, l, 1)
            go(mlstm_stage, s, l)
            if dbg and s == 0 and l == 0:
                for bi, nm in enumerate(("dbg_ya", "dbg_yb", "dbg_yc")):
                    dump(nm, yT[bi].rearrange("p c n -> p (c n)"), [x for y in b_yT[bi] for x in y], finals)
                P.barrier()
            go(merge_stage, s, l, finals)
            P.barrier(new_epoch=True)
    P.emit(final_waits=finals)
    return nc


_CACHE = {}


def prep_inputs(inputs):
    f = np.float32
    norm_g = np.asarray(inputs["norm_g"], f)
    gfull = np.zeros((128, DEPTH, 8, 128), f)
    for l in range(DEPTH):
        gfull[:, l, :, :] = norm_g[l].reshape(8, 128).T[:, :, None]
    shared = {
        "w_in": np.ascontiguousarray(inputs["w_in"], f),
        "w_branch": np.ascontiguousarray(inputs["w_branch"], f),
        "w_out": np.ascontiguousarray(inputs["w_out"], f),
        "gfull": gfull.reshape(128, -1),
        "fgrep": np.ascontiguousarray(np.broadcast_to(np.asarray(inputs["final_norm_g"], f)[None, :], (128, D))),
    }
    fbf = np.zeros((128, DEPTH, NT, 8), f)
    bif = np.zeros((128, DEPTH, NT, 8), f)
    for l in range(DEPTH):
        fbf[:, l, :, :] = np.asarray(inputs["fox_b_f"], f)[l][None, None, :]
        bif[:, l, :, 0:4] = np.asarray(inputs["mlstm_b_i"], f)[l][None, None, :]
        bif[:, l, :, 4:8] = np.asarray(inputs["mlstm_b_f"], f)[l][None, None, :]
    shared["fbf_rep"] = fbf.reshape(128, -1)
    shared["bif_rep"] = bif.reshape(128, -1)
    cwv = np.asarray(inputs["mlstm_conv_w"], f)
    cw = np.zeros((128, DEPTH, 4, 4), f)
    for l in range(DEPTH):
        cw[:, l, :, :] = cwv[l].reshape(4, 4, 128).transpose(2, 1, 0)
    shared["cw"] = cw.reshape(128, -1)
    hgv = np.asarray(inputs["mlstm_head_g"], f)
    hg = np.zeros((128, DEPTH, 4), f)
    for l in range(DEPTH):
        hg[:, l, :] = hgv[l].reshape(4, 128).T
    shared["hg"] = hg.reshape(128, -1)
    for k, v in host_consts().items():
        shared["c_" + k] = np.ascontiguousarray(v, f)
    return shared


def kernel(**inputs):
    x = np.ascontiguousarray(inputs["x"], np.float32)
    shared = prep_inputs(inputs)
    if "nc" not in _CACHE:
        _CACHE["nc"] = build(False)
    nc = _CACHE["nc"]
    in_maps = []
    for c in range(8):
        m = dict(shared)
        m["x"] = np.ascontiguousarray(x[2 * c:2 * c + 2])
        in_maps.append(m)
    res = run_bass_kernel_spmd(nc, in_maps, core_ids=list(range(8)))
    out = np.concatenate([np.asarray(r["out"], np.float32) for r in res.results], axis=0)
    return out
```
